# Optimizing a Trainium2 kernel written in Bass

```python
import math
import jax
import jax.numpy as jnp
from jax import lax
import numpy as np

D_MODEL = 1024
BATCH = 4
SEQ = 8192
DEPTH = 2

GRID_W = 64
CTX_LEN = 256
EPS = 1e-6

DN_HEADS = 8
DN_HEAD_DIM = 64
DN_DIM = DN_HEADS * DN_HEAD_DIM
DN_CONV_W = 5
DN_CHUNK = 64

AT_Q_HEADS = 8
AT_KV_HEADS = 2
AT_GROUP = AT_Q_HEADS // AT_KV_HEADS
AT_HEAD_DIM = 64
AT_Q_DIM = AT_Q_HEADS * AT_HEAD_DIM
AT_KV_DIM = AT_KV_HEADS * AT_HEAD_DIM
Q_BLOCK = 128
ROPE_THETA = 10000.0
ROPE_AXIS_DIM = AT_HEAD_DIM // 2

FFN_DIM = 2816
N_EXPERTS = 8
TOP_K = 2
EXPERT_DIM = 3584
MOE_BLOCK = 256
N_DENSE = (DEPTH + 1) // 2
N_MOE = DEPTH // 2

OFF_DN_Z = 3 * DN_DIM
OFF_DN_A = OFF_DN_Z + DN_DIM
OFF_DN_B = OFF_DN_A + 2 * DN_HEADS
OFF_AT_Q = OFF_DN_B + 2 * DN_HEADS
OFF_AT_K = OFF_AT_Q + AT_Q_DIM
OFF_AT_V = OFF_AT_K + AT_KV_DIM
IN_DIM = OFF_AT_V + AT_KV_DIM
MIX_DIM = DN_DIM + AT_Q_DIM

kernel_name = 'hybrid_deltanet_axial_gqa_moe_prefix_dit'


def _rmsnorm(x, w):
    x32 = x.astype(jnp.float32)
    y = x32 * lax.rsqrt(jnp.mean(x32 * x32, axis=-1, keepdims=True) + EPS)
    return (y * w.astype(jnp.float32)).astype(x.dtype)


def _modulate(h, shift, scale):
    return h * (1.0 + scale) + shift


def _l2norm(x):
    return x * lax.rsqrt(jnp.sum(x * x, axis=-1, keepdims=True) + EPS)


def _centred_dwconv(x, w):
    pad = (w.shape[0] - 1) // 2
    return lax.conv_general_dilated(
        x, w[:, None, :], window_strides=(1,), padding=[(pad, pad)],
        dimension_numbers=('NWC', 'WIO', 'NWC'), feature_group_count=x.shape[-1])


def _axial_rope_tables(t):
    rows = t // GRID_W
    row_pos = jnp.repeat(jnp.arange(rows, dtype=jnp.float32), GRID_W, total_repeat_length=t)
    col_pos = jnp.tile(jnp.arange(GRID_W, dtype=jnp.float32), rows)
    n_freq = ROPE_AXIS_DIM // 2
    freqs = ROPE_THETA ** (-jnp.arange(n_freq, dtype=jnp.float32) / n_freq)
    ang = jnp.concatenate([row_pos[:, None] * freqs, col_pos[:, None] * freqs], axis=-1)
    return jnp.cos(ang), jnp.sin(ang)


def _apply_rope(x, cos, sin):
    half = x.shape[-1] // 2
    x32 = x.astype(jnp.float32)
    x1, x2 = x32[..., :half], x32[..., half:]
    cos, sin = cos[None, :, None, :], sin[None, :, None, :]
    return jnp.concatenate([x1 * cos - x2 * sin, x2 * cos + x1 * sin], axis=-1).astype(x.dtype)


def _gated_delta_chunked(q, k, v, g, beta, s0):
    b, h, t, dk = q.shape
    dv = v.shape[-1]
    n = t // DN_CHUNK
    chunk = lambda z: z.reshape((b, h, n, DN_CHUNK) + z.shape[3:])
    q, k, v, g, beta = chunk(q), chunk(k), chunk(v), chunk(g), chunk(beta)
    g = jnp.cumsum(g, axis=-1)
    incl = jnp.tril(jnp.ones((DN_CHUNK, DN_CHUNK), dtype=bool))
    strict = jnp.tril(jnp.ones((DN_CHUNK, DN_CHUNK), dtype=bool), -1)
    diff = g[..., :, None] - g[..., None, :]
    decay = jnp.where(incl, jnp.exp(jnp.where(incl, diff, 0.0)), 0.0)
    k_beta = k * beta[..., None]
    a_low = jnp.where(strict, jnp.einsum('bhncd,bhnsd->bhncs', k_beta, k) * decay, 0.0)
    rhs = jnp.concatenate([v * beta[..., None], k_beta * jnp.exp(g)[..., None]], axis=-1)
    eye = jnp.eye(DN_CHUNK, dtype=q.dtype)
    sol = lax.linalg.triangular_solve(a_low + eye, rhs, left_side=True, lower=True, unit_diagonal=True)
    u, w = sol[..., :dv], sol[..., dv:]
    attn = jnp.where(incl, jnp.einsum('bhncd,bhnsd->bhncs', q, k) * decay, 0.0)
    q_dec = q * jnp.exp(g)[..., None]
    k_dec = k * jnp.exp(g[..., -1:] - g)[..., None]
    g_last = jnp.exp(g[..., -1])

    def step(s, xs):
        u_c, w_c, attn_c, q_c, k_c, gl_c = xs
        v_new = u_c - jnp.einsum('bhcd,bhde->bhce', w_c, s)
        o_c = jnp.einsum('bhcd,bhde->bhce', q_c, s) + jnp.einsum('bhcs,bhse->bhce', attn_c, v_new)
        s = s * gl_c[..., None, None] + jnp.einsum('bhcd,bhce->bhde', k_c, v_new)
        return s, o_c

    xs = tuple(jnp.moveaxis(z, 2, 0) for z in (u, w, attn, q_dec, k_dec, g_last))
    s_final, o = lax.scan(step, s0, xs)
    o = jnp.moveaxis(o, 0, 2).reshape(b, h, t, dv)
    return o, s_final


def _dn_streams(p, conv_w, a_log, dt_bias):
    b, t, _ = p.shape
    p = p.astype(jnp.float32)
    qkv = jax.nn.silu(_centred_dwconv(p[..., :3 * DN_DIM], conv_w.astype(jnp.float32)))
    q, k, v = jnp.split(qkv, 3, axis=-1)
    heads = lambda z: z.reshape(b, t, DN_HEADS, DN_HEAD_DIM).transpose(0, 2, 1, 3)
    q = _l2norm(heads(q)) * (DN_HEAD_DIM ** -0.5)
    k = _l2norm(heads(k))
    v = heads(v)
    a = p[..., OFF_DN_A:OFF_DN_A + 2 * DN_HEADS].reshape(b, t, 2, DN_HEADS)
    bb = p[..., OFF_DN_B:OFF_DN_B + 2 * DN_HEADS].reshape(b, t, 2, DN_HEADS)
    g = -jnp.exp(a_log.astype(jnp.float32)) * jax.nn.softplus(a + dt_bias.astype(jnp.float32))
    beta = jax.nn.sigmoid(bb)
    return q, k, v, g.transpose(2, 0, 3, 1), beta.transpose(2, 0, 3, 1)


def _bidir_delta(q, k, v, g, beta, s0_f, s0_b):
    flip = lambda z: jnp.flip(z, axis=2)
    o_f, s_f = _gated_delta_chunked(q, k, v, g[0], beta[0], s0_f)
    o_b, s_b = _gated_delta_chunked(flip(q), flip(k), flip(v), flip(g[1]), flip(beta[1]), s0_b)
    return o_f + flip(o_b), s_f, s_b


def _dn_output(o, p, norm_w):
    b, h, t, dv = o.shape
    o = o.transpose(0, 2, 1, 3)
    o = o * lax.rsqrt(jnp.mean(o * o, axis=-1, keepdims=True) + EPS) * norm_w.astype(jnp.float32)
    z = p[..., OFF_DN_Z:OFF_DN_Z + DN_DIM].astype(jnp.float32).reshape(b, t, h, dv)
    return (o * jax.nn.silu(z)).reshape(b, t, DN_DIM).astype(p.dtype)


def _gated_deltanet(p_lat, p_ctx, conv_w, a_log, dt_bias, norm_w, need_ctx_out):
    b = p_lat.shape[0]
    zero = jnp.zeros((b, DN_HEADS, DN_HEAD_DIM, DN_HEAD_DIM), jnp.float32)
    qc, kc, vc, gc, bc = _dn_streams(p_ctx, conv_w, a_log, dt_bias)
    o_c, s_f, s_b = _bidir_delta(qc, kc, vc, gc, bc, zero, zero)
    q, k, v, g, be = _dn_streams(p_lat, conv_w, a_log, dt_bias)
    o, _, _ = _bidir_delta(q, k, v, g, be, s_f, s_b)
    lat = _dn_output(o, p_lat, norm_w)
    ctx = _dn_output(o_c, p_ctx, norm_w) if need_ctx_out else None
    return lat, ctx


def _attend(qb, kk, vv):
    s = jnp.einsum('bqkgd,bskd->bkgqs', qb, kk).astype(jnp.float32) * (AT_HEAD_DIM ** -0.5)
    pr = jax.nn.softmax(s, axis=-1).astype(vv.dtype)
    return jnp.einsum('bkgqs,bskd->bqkgd', pr, vv)


def _axial_gqa(p_lat, p_ctx, qn_w, kn_w, need_ctx_out):
    b, t, _ = p_lat.shape
    lc = p_ctx.shape[1]
    q_of = lambda p: _rmsnorm(p[..., OFF_AT_Q:OFF_AT_Q + AT_Q_DIM].reshape(p.shape[0], p.shape[1], AT_Q_HEADS, AT_HEAD_DIM), qn_w)
    k_of = lambda p: _rmsnorm(p[..., OFF_AT_K:OFF_AT_K + AT_KV_DIM].reshape(p.shape[0], p.shape[1], AT_KV_HEADS, AT_HEAD_DIM), kn_w)
    v_of = lambda p: p[..., OFF_AT_V:OFF_AT_V + AT_KV_DIM].reshape(p.shape[0], p.shape[1], AT_KV_HEADS, AT_HEAD_DIM)
    cos, sin = _axial_rope_tables(t)
    q = _apply_rope(q_of(p_lat), cos, sin)
    k = _apply_rope(k_of(p_lat), cos, sin)
    kc, vc = k_of(p_ctx), v_of(p_ctx)
    k_all = jnp.concatenate([k, kc], axis=1)
    v_all = jnp.concatenate([v_of(p_lat), vc], axis=1)
    nb = t // Q_BLOCK
    qb = q.reshape(b, nb, Q_BLOCK, AT_KV_HEADS, AT_GROUP, AT_HEAD_DIM).transpose(1, 0, 2, 3, 4, 5)
    o = lax.map(lambda blk: _attend(blk, k_all, v_all), qb)
    lat = o.transpose(1, 0, 2, 3, 4, 5).reshape(b, t, AT_Q_DIM)
    ctx = None
    if need_ctx_out:
        qc = q_of(p_ctx).reshape(b, lc, AT_KV_HEADS, AT_GROUP, AT_HEAD_DIM)
        ctx = _attend(qc, kc, vc).reshape(b, lc, AT_Q_DIM)
    return lat, ctx


def _swiglu(h, w_gate, w_up, w_down):
    return (jax.nn.silu(h @ w_gate) * (h @ w_up)) @ w_down


def _moe_swiglu(h, router_w, router_b, w_gate, w_up, w_down):
    b, l, d = h.shape
    hf = h.reshape(-1, d)
    n = hf.shape[0]
    logits = (hf @ router_w).astype(jnp.float32) + router_b.astype(jnp.float32)
    top_val, top_e = lax.top_k(logits, TOP_K)
    top_w = jax.nn.softmax(top_val, axis=-1)
    m = n * TOP_K
    flat_e = top_e.reshape(m)
    flat_tok = jnp.repeat(jnp.arange(n, dtype=jnp.int32), TOP_K)
    flat_w = top_w.reshape(m)
    order = jnp.argsort(flat_e, stable=True)
    sorted_e = flat_e[order]
    counts = jnp.bincount(flat_e, length=N_EXPERTS)
    padded = (counts + MOE_BLOCK - 1) // MOE_BLOCK * MOE_BLOCK
    start = jnp.cumsum(counts) - counts
    pad_end = jnp.cumsum(padded)
    pad_start = pad_end - padded
    dest = pad_start[sorted_e] + jnp.arange(m, dtype=jnp.int32) - start[sorted_e]
    p_rows = ((m + MOE_BLOCK - 1) // MOE_BLOCK + N_EXPERTS) * MOE_BLOCK
    row_tok = jnp.full((p_rows,), n, jnp.int32).at[dest].set(flat_tok[order])
    row_w = jnp.zeros((p_rows,), jnp.float32).at[dest].set(flat_w[order])
    nblk = p_rows // MOE_BLOCK
    blk_e = jnp.minimum(jnp.searchsorted(pad_end, jnp.arange(nblk, dtype=pad_end.dtype) * MOE_BLOCK, side='right'), N_EXPERTS - 1)
    xs = jnp.concatenate([hf, jnp.zeros((1, d), hf.dtype)], axis=0)[row_tok].reshape(nblk, MOE_BLOCK, d)

    def expert_block(args):
        xb, e = args
        return _swiglu(xb, w_gate[e], w_up[e], w_down[e])

    ys = lax.map(expert_block, (xs, blk_e)).reshape(p_rows, d)
    out = jnp.zeros((n + 1, d), ys.dtype).at[row_tok].add(ys * row_w[:, None].astype(ys.dtype))
    return out[:n].reshape(b, l, d)


def setup_inputs(seed: int = 0) -> dict:
    key = jax.random.key(seed)
    ks = jax.random.split(key, 32)
    f32 = jnp.float32
    D, L = D_MODEL, DEPTH
    nrm = lambda k, shape, s: jax.random.normal(k, shape, f32) * s
    dt = jnp.exp(jax.random.uniform(ks[10], (L, 2, DN_HEADS), f32, math.log(1e-3), math.log(1e-1)))
    return {
        'x': nrm(ks[0], (BATCH, SEQ, D), 1.0),
        'c': nrm(ks[1], (BATCH, D), 1.0),
        'ctx': nrm(ks[2], (BATCH, CTX_LEN, D), 1.0),
        'c_ctx': nrm(ks[3], (D,), 1.0),
        'w_mod': nrm(ks[4], (L, D, 6 * D), 0.5 * D ** -0.5),
        'b_mod': nrm(ks[5], (L, 6 * D), 0.02),
        'norm1_w': 1.0 + nrm(ks[6], (L, D), 0.02),
        'norm2_w': 1.0 + nrm(ks[7], (L, D), 0.02),
        'w_in': nrm(ks[8], (L, D, IN_DIM), D ** -0.5),
        'dn_conv_w': nrm(ks[9], (L, DN_CONV_W, 3 * DN_DIM), DN_CONV_W ** -0.5),
        'dn_a_log': jnp.log(jax.random.uniform(ks[11], (L, 2, DN_HEADS), f32, 1.0, 16.0)),
        'dn_dt_bias': dt + jnp.log(-jnp.expm1(-dt)),
        'dn_norm_w': 1.0 + nrm(ks[12], (L, DN_HEAD_DIM), 0.02),
        'at_qnorm_w': 1.0 + nrm(ks[13], (L, AT_HEAD_DIM), 0.02),
        'at_knorm_w': 1.0 + nrm(ks[14], (L, AT_HEAD_DIM), 0.02),
        'w_out': nrm(ks[15], (L, MIX_DIM, D), MIX_DIM ** -0.5),
        'ffn_w_gate': nrm(ks[16], (N_DENSE, D, FFN_DIM), D ** -0.5),
        'ffn_w_up': nrm(ks[17], (N_DENSE, D, FFN_DIM), D ** -0.5),
        'ffn_w_down': nrm(ks[18], (N_DENSE, FFN_DIM, D), FFN_DIM ** -0.5),
        'moe_router_w': nrm(ks[19], (N_MOE, D, N_EXPERTS), D ** -0.5),
        'moe_router_b': nrm(ks[20], (N_MOE, N_EXPERTS), 0.01),
        'moe_w_gate': nrm(ks[21], (N_MOE, N_EXPERTS, D, EXPERT_DIM), D ** -0.5),
        'moe_w_up': nrm(ks[22], (N_MOE, N_EXPERTS, D, EXPERT_DIM), D ** -0.5),
        'moe_w_down': nrm(ks[23], (N_MOE, N_EXPERTS, EXPERT_DIM, D), EXPERT_DIM ** -0.5),
        'final_norm_w': 1.0 + nrm(ks[24], (D,), 0.02),
    }


def reference(x, c, ctx, c_ctx, w_mod, b_mod, norm1_w, norm2_w, w_in, dn_conv_w, dn_a_log,
              dn_dt_bias, dn_norm_w, at_qnorm_w, at_knorm_w, w_out, ffn_w_gate, ffn_w_up,
              ffn_w_down, moe_router_w, moe_router_b, moe_w_gate, moe_w_up, moe_w_down,
              final_norm_w):
    xc = ctx
    sc = jax.nn.silu(c)
    scc = jax.nn.silu(c_ctx)
    for layer in range(DEPTH):
        last = layer == DEPTH - 1
        mod = (sc @ w_mod[layer] + b_mod[layer])[:, None, :]
        mod_c = scc @ w_mod[layer] + b_mod[layer]
        sh1, sc1, g1, sh2, sc2, g2 = jnp.split(mod, 6, axis=-1)
        sh1c, sc1c, g1c, sh2c, sc2c, g2c = jnp.split(mod_c, 6, axis=-1)
        p = _modulate(_rmsnorm(x, norm1_w[layer]), sh1, sc1) @ w_in[layer]
        pc = _modulate(_rmsnorm(xc, norm1_w[layer]), sh1c, sc1c) @ w_in[layer]
        dn, dn_c = _gated_deltanet(p, pc, dn_conv_w[layer], dn_a_log[layer], dn_dt_bias[layer],
                                   dn_norm_w[layer], not last)
        at, at_c = _axial_gqa(p, pc, at_qnorm_w[layer], at_knorm_w[layer], not last)
        x = x + g1 * (jnp.concatenate([dn, at], axis=-1) @ w_out[layer])
        if layer % 2 == 0:
            i = layer // 2
            ffn = lambda t: _swiglu(t, ffn_w_gate[i], ffn_w_up[i], ffn_w_down[i])
        else:
            j = layer // 2
            ffn = lambda t: _moe_swiglu(t, moe_router_w[j], moe_router_b[j], moe_w_gate[j],
                                        moe_w_up[j], moe_w_down[j])
        x = x + g2 * ffn(_modulate(_rmsnorm(x, norm2_w[layer]), sh2, sc2))
        if not last:
            xc = xc + g1c * (jnp.concatenate([dn_c, at_c], axis=-1) @ w_out[layer])
            xc = xc + g2c * ffn(_modulate(_rmsnorm(xc, norm2_w[layer]), sh2c, sc2c))
    return _rmsnorm(x, final_norm_w)
```

```python
import contextlib
import numpy as np
import concourse.bass as bass
import concourse.mybir as mybir

F32 = mybir.dt.float32
BF16 = mybir.dt.bfloat16
ALU = mybir.AluOpType
AF = mybir.ActivationFunctionType
AX = mybir.AxisListType


class Res:
    __slots__ = ("name", "lw", "rd")

    def __init__(self, name=""):
        self.name = name
        self.lw = None
        self.rd = {}


class Prog:
    ENG = ("pe", "act", "dve", "pool", "sp")
    NDMA = 10

    def __init__(self, nc):
        self.nc = nc
        self.es = contextlib.ExitStack()
        self.q = {e: [] for e in self.ENG}
        self.cnt = {e: 0 for e in self.ENG}
        self.sems = {}
        for e in self.ENG:
            self.sems[e] = self.es.enter_context(nc.semaphore("c_" + e))
        self.dcnt = {}
        self.dnext = {}
        for qn in ("sp", "pool", "act"):
            for i in range(self.NDMA):
                k = "d_%s_%d" % (qn, i)
                self.sems[k] = self.es.enter_context(nc.semaphore(k))
                self.dcnt[k] = 0
            self.dnext[qn] = 0
        self.waited = {}
        self.ntile = 0
        self.ninst = 0

    def sb(self, shape, dt, name=None):
        self.ntile += 1
        name = name or ("t%d" % self.ntile)
        return self.es.enter_context(self.nc.sbuf_tensor(name, list(shape), dt))

    def ps(self, shape, dt, name=None):
        self.ntile += 1
        name = name or ("p%d" % self.ntile)
        return self.es.enter_context(self.nc.psum_tensor(name, list(shape), dt))

    def _need(self, eng, dep, out):
        if dep is None:
            return
        key, val = dep
        if key == "pe" and eng == "pe":
            return
        if self.waited.get((eng, key), 0) >= val:
            return
        self.waited[(eng, key)] = val
        out.append((key, val))

    def _deps(self, eng, reads, writes):
        out = []
        for r in reads:
            self._need(eng, r.lw, out)
        for w in writes:
            self._need(eng, w.lw, out)
            for k, d in w.rd.items():
                self._need(eng, d, out)
        return out

    def _emit_waits(self, eng, deps):
        sems = self.sems
        for key, val in deps:
            self.q[eng].append(lambda h, key=key, val=val: h.wait_ge(sems[key], val))
            self.ninst += 1

    def op(self, eng, meth, *args, reads=(), writes=(), inc=True, **kw):
        deps = self._deps(eng, reads, writes)
        self._emit_waits(eng, deps)
        self.ninst += 1
        if inc:
            self.cnt[eng] += 1
            val = self.cnt[eng]
            sem = self.sems[eng]
            self.q[eng].append(lambda h: getattr(h, meth)(*args, **kw).then_inc(sem, 1))
        else:
            val = self.cnt[eng] + 1
            self.q[eng].append(lambda h: getattr(h, meth)(*args, **kw))
        dep = (eng, val)
        for r in reads:
            r.rd[eng] = dep
        for w in writes:
            w.lw = dep
            w.rd = {}
        return dep

    def dma(self, qn, out, in_, reads=(), writes=(), **kw):
        i = self.dnext[qn]
        self.dnext[qn] = (i + 1) % self.NDMA
        k = "d_%s_%d" % (qn, i)
        deps = self._deps(qn, reads, writes)
        if self.dcnt[k] > 0:
            self._need(qn, (k, self.dcnt[k]), deps)
        self._emit_waits(qn, deps)
        self.dcnt[k] += 16
        val = self.dcnt[k]
        sem = self.sems[k]
        self.ninst += 1
        self.q[qn].append(lambda h: h.dma_start(out=out, in_=in_, **kw).then_inc(sem, 16))
        dep = (k, val)
        for r in reads:
            r.rd[k] = dep
        for w in writes:
            w.lw = dep
            w.rd = {}
        return dep

    def wait_all(self, eng, resources):
        out = []
        for r in resources:
            self._need(eng, r.lw, out)
        self._emit_waits(eng, out)

    def barrier(self):
        for e in self.ENG:
            out = []
            for e2 in self.ENG:
                if e2 != e and self.cnt[e2] > 0:
                    self._need(e, (e2, self.cnt[e2]), out)
            for k, v in self.dcnt.items():
                if v > 0:
                    self._need(e, (k, v), out)
            self._emit_waits(e, out)

    def barrier_cc(self):
        if "cc" in self.sems and getattr(self, "cccnt", 0) > 0:
            for e in self.ENG:
                out = []
                self._need(e, ("cc", self.cccnt), out)
                self._emit_waits(e, out)
        self.barrier()

    def finish(self):
        nc = self.nc
        q = self.q
        with nc.Block() as block:
            @block.tensor
            def _(h):
                for f in q["pe"]:
                    f(h)

            @block.scalar
            def _(h):
                for f in q["act"]:
                    f(h)

            @block.vector
            def _(h):
                for f in q["dve"]:
                    f(h)

            @block.gpsimd
            def _(h):
                for f in q["pool"]:
                    f(h)

            @block.sync
            def _(h):
                for f in q["sp"]:
                    f(h)
        self.es.close()


class Arena:
    def __init__(self, P, nbytes, name="arena"):
        self.t = P.sb([128, nbytes // 4], F32, name=name)
        self.nbytes = nbytes
        self.off = 0
        self.peak = 0

    def alloc(self, free_shape, dt, parts=128):
        esz = 4 if dt == F32 else 2
        n = int(np.prod(free_shape))
        nb = (n * esz + 31) // 32 * 32
        assert self.off + nb <= self.nbytes, "arena overflow %d + %d > %d" % (self.off, nb, self.nbytes)
        w0 = self.off // 4
        ap = self.t[0:parts, w0:w0 + nb // 4]
        if dt != F32:
            ap = ap.bitcast(dt)
        ap = ap[:, 0:n]
        self.off += nb
        self.peak = max(self.peak, self.off)
        if len(free_shape) == 2:
            ap = ap.rearrange("p (a b) -> p a b", a=free_shape[0])
        elif len(free_shape) == 3:
            ap = ap.rearrange("p (a b c) -> p a b c", a=free_shape[0], b=free_shape[1])
        return ap

    def mark(self):
        return self.off

    def release(self, m):
        self.off = m


D = 1024
NCTX = 256
EPS = 1e-6
BIG = 30000.0


def emit_mod(P, A, PS, dram, nchunk, consts):
    ccT = A.alloc([8, 2], F32); r_cc = Res()
    P.dma("sp", ccT, dram["ccT"], writes=[r_cc])
    P.op("act", "activation", ccT, ccT, AF.Silu, reads=[r_cc], writes=[r_cc])
    modrow = A.alloc([nchunk * 1024], F32, parts=2); r_mod = Res()
    m0 = A.mark()
    wbuf = [A.alloc([8, 512], F32) for _ in range(2)]
    bmb = [A.alloc([512], F32, parts=2) for _ in range(2)]
    r_w = [Res(), Res()]
    r_bm = [Res(), Res()]
    r_ps = [Res(), Res()]
    for g in range(nchunk * 2):
        wb = wbuf[g % 2]
        c0 = g * 512
        P.dma("sp", wb, dram["wmod"][:, c0:c0 + 512].rearrange("(c k) n -> k c n", k=128), writes=[r_w[g % 2]])
        P.dma("sp", bmb[g % 2], dram["bmod"][:, c0:c0 + 512], writes=[r_bm[g % 2]])
        pb = PS[0:2, (g % 2) * 512:(g % 2 + 1) * 512]
        for kc in range(8):
            P.op("pe", "matmul", pb, ccT[:, kc, :], wb[:, kc, :],
                 start=(kc == 0), stop=(kc == 7), reads=[r_cc, r_w[g % 2]], writes=[r_ps[g % 2]], inc=(kc == 7))
        P.op("dve", "tensor_tensor", modrow[:, c0:c0 + 512], pb, bmb[g % 2], ALU.add,
             reads=[r_ps[g % 2], r_bm[g % 2]], writes=[r_mod])
    P.barrier()
    A.release(m0)
    return modrow, r_mod


def emit_bcast(P, PS, r_psb, sel, r_sel, row_ap, r_row, out_ap, r_out, n=1024, bank=0):
    for g in range(n // 512):
        pb = PS[:, bank * 512:(bank + 1) * 512]
        P.op("pe", "matmul", pb, sel, row_ap[:, g * 512:(g + 1) * 512], start=True, stop=True,
             reads=[r_sel, r_row], writes=[r_psb])
        P.op("dve", "tensor_copy", out_ap[:, g * 512:(g + 1) * 512], pb, reads=[r_psb], writes=[r_out])


def emit_sel(P, A):
    sel0 = A.alloc([128], F32, parts=2)
    sel1 = A.alloc([128], F32, parts=2)
    r = Res()
    P.op("dve", "memset", sel0, 0.0, writes=[r])
    P.op("dve", "memset", sel0[0:1, :], 1.0, writes=[r])
    P.op("dve", "memset", sel1, 1.0, writes=[r])
    P.op("dve", "memset", sel1[0:1, :], 0.0, writes=[r])
    return sel0, sel1, r


def emit_rstd(P, ss, r_ss, out, r_out, scale, eps_ap, r_eps, premul=None):
    P.op("act", "activation", out, ss, AF.Ln, bias=eps_ap, scale=scale, reads=[r_ss, r_eps], writes=[r_out])
    P.op("act", "activation", out, out, AF.Exp, scale=-0.5, reads=[r_out], writes=[r_out])


def emit_M(P, A, PS, dr, T, want_ctx_out, xrow, mix, Zd, REC, do_attn=True, do_dn=True, dn_stage=9):
    debug = False
    dbg = None
    NT = T + NCTX
    nkt = NT // 128
    P.barrier()
    A.release(0)

    def bank(i, n=512):
        return PS[:, i * 512:i * 512 + n]

    def bankb(i, n=1024):
        return PS[:, i * 512:(i + 1) * 512].bitcast(BF16)[:, 0:n]

    ident = A.alloc([128], F32); r_ident = Res()
    identb = A.alloc([128], BF16); r_identb = Res()
    P.dma("sp", ident, dr["ident"], writes=[r_ident])
    P.dma("pool", identb, dr["ident"], writes=[r_identb])
    epst = A.alloc([1], F32); r_eps = Res()
    P.op("dve", "memset", epst, EPS, writes=[r_eps])

    GB = A.alloc([nkt, 16], F32); r_GB = Res()
    m_att = A.mark()
    QT = A.alloc([2, T], BF16); r_QT = Res()
    KT = A.alloc([NT], BF16); r_KT = Res()
    VA = A.alloc([nkt, 65], BF16); r_VA = Res()
    QTc = A.alloc([2, NCTX], BF16); r_QTc = Res()
    P.op("pool", "memset", VA, 1.0, writes=[r_VA])
    m_p1 = A.mark()

    sel0, sel1, r_sel = emit_sel(P, A)
    modrow, r_mod = emit_mod(P, A, PS, dr, 2, None)
    nwrow = A.alloc([D], F32, parts=2); r_nw = Res()
    P.dma("sp", nwrow, dr["nw"], writes=[r_nw])
    weff = A.alloc([D], F32); r_weff = Res()
    shb = A.alloc([D], F32); r_shb = Res()
    r_psb = Res()

    def make_mod_tiles(sel):
        emit_bcast(P, PS, r_psb, sel, r_sel, modrow[:, 1024:2048], r_mod, weff, r_weff, bank=2)
        P.op("dve", "tensor_scalar", weff, weff, 1.0, None, ALU.add, reads=[r_weff], writes=[r_weff])
        emit_bcast(P, PS, r_psb, sel, r_sel, nwrow, r_nw, shb, r_shb, bank=2)
        P.op("dve", "tensor_tensor", weff, weff, shb, ALU.mult, reads=[r_weff, r_shb], writes=[r_weff])
        emit_bcast(P, PS, r_psb, sel, r_sel, modrow[:, 0:1024], r_mod, shb, r_shb, bank=2)

    Wtm = A.alloc([8, 656], BF16); r_Wtm = Res()
    Wfm = A.alloc([8, 768], BF16); r_Wfm = Res()
    P.dma("pool", Wtm, dr["wtm"].rearrange("(c k) n -> k c n", k=128), writes=[r_Wtm])
    P.dma("pool", Wfm, dr["wfm"].rearrange("(c k) n -> k c n", k=128), writes=[r_Wfm])
    convw = A.alloc([6, 5], F32); r_convw = Res()
    P.dma("sp", convw, dr["convw"], writes=[r_convw])
    gp = A.alloc([16], F32); r_gp = Res()
    P.dma("sp", gp, dr["gp"], writes=[r_gp])
    P.op("act", "activation", gp[:, 8:16], gp[:, 8:16], AF.Exp, reads=[r_gp], writes=[r_gp])
    P.op("dve", "tensor_scalar", gp[:, 8:16], gp[:, 8:16], -1.0, None, ALU.mult, reads=[r_gp], writes=[r_gp])
    qkw = A.alloc([5, 64], F32); r_qkw = Res()
    P.dma("sp", qkw, dr["qkw"].rearrange("p (a b) -> p a b", a=5), writes=[r_qkw])

    xt = [A.alloc([D], F32) for _ in range(2)]; r_xt = [Res(), Res()]
    htmp = A.alloc([D], F32); r_htmp = Res()
    hb = A.alloc([D], BF16); r_hb = Res()
    hT = A.alloc([8, 512], BF16); r_hT = Res()
    XR = [A.alloc([6, 516], F32) for _ in range(2)]; r_XR = [Res(), Res()]
    cacc = A.alloc([6, 512], F32); r_cacc = Res()
    sact = A.alloc([6, 512], BF16); r_sact = Res()
    tms = A.alloc([656], F32); r_tms = Res()
    ss1 = A.alloc([1], F32); r_ss1 = Res()
    rs1 = A.alloc([1], F32); r_rs1 = Res()
    junk = A.alloc([D], F32)
    r_junk = Res()
    sq5 = A.alloc([5, 64], F32); r_sq5 = Res()
    ss5 = A.alloc([5], F32); r_ss5 = Res()
    rs5 = A.alloc([5], F32); r_rs5 = Res()
    qn = A.alloc([5, 64], F32); r_qn = Res()
    tt = [A.alloc([5, 32], F32) for _ in range(4)]; r_tt = Res()
    qr = A.alloc([6, 64], BF16); r_qr = Res()
    cst = A.alloc([2, 32], F32); r_cst = Res()
    gt = A.alloc([16], F32); r_gt = Res()
    rec = A.alloc([768], F32); r_rec = Res()
    sq8 = A.alloc([512], F32); r_sq8 = Res()
    ss8 = A.alloc([8], F32); r_ss8 = Res()
    rs8 = A.alloc([8], F32); r_rs8 = Res()

    r_pT = [Res(), Res()]
    r_ptm = Res()
    r_pfm = [Res(), Res()]
    r_pat = Res()
    r_pdn = Res()
    r_Zd = Res(); r_REC = Res(); r_dbg = Res()

    macros = [(0, NCTX, True)] + [(NCTX + i * 512, 512, False) for i in range(T // 512)]
    nsub_done = 0

    def conv_macro(mi):
        s0, n, is_ctx = macros[mi]
        X = XR[mi % 2]; rX = r_XR[mi % 2]
        for ch in range(6):
            eng = "dve"
            P.op(eng, "tensor_scalar", cacc[:, ch, 0:n], X[:, ch, 0:n], convw[:, ch, 0:1], None, ALU.mult,
                 reads=[rX, r_convw], writes=[r_cacc])
            for k in range(1, 5):
                P.op(eng, "scalar_tensor_tensor", cacc[:, ch, 0:n], X[:, ch, k:k + n], convw[:, ch, k:k + 1],
                     cacc[:, ch, 0:n], ALU.mult, ALU.add, reads=[rX, r_convw, r_cacc], writes=[r_cacc])
        P.op("act", "activation", sact[:, :, 0:n], cacc[:, :, 0:n], AF.Silu, reads=[r_cacc], writes=[r_sact])
        for sub in range(n // 128):
            t0 = s0 + sub * 128
            pb = bankb(7, 768)
            for ch in range(6):
                P.op("pe", "transpose", pb[:, ch * 128:(ch + 1) * 128], sact[:, ch, sub * 128:(sub + 1) * 128], identb,
                     reads=[r_sact, r_identb], writes=[r_pdn], inc=(ch == 5))
            P.op("act", "activation", rec, pb, AF.Copy, reads=[r_pdn], writes=[r_rec])
            P.op("dve", "tensor_tensor", sq8, rec[:, 0:512], rec[:, 0:512], ALU.mult, reads=[r_rec], writes=[r_sq8])
            P.op("dve", "tensor_reduce", ss8, sq8.rearrange("p (a b) -> p a b", a=8), AX.X, ALU.add,
                 reads=[r_sq8], writes=[r_ss8])
            emit_rstd(P, ss8, r_ss8, rs8, r_rs8, 1.0, epst, r_eps)
            P.op("dve", "tensor_scalar", rs8[:, 0:4], rs8[:, 0:4], 0.125, None, ALU.mult, reads=[r_rs8], writes=[r_rs8])
            P.op("dve", "tensor_tensor", rec[:, 0:512].rearrange("p (a b) -> p a b", a=8),
                 rec[:, 0:512].rearrange("p (a b) -> p a b", a=8),
                 rs8.unsqueeze(2).to_broadcast([128, 8, 64]), ALU.mult, reads=[r_rec, r_rs8], writes=[r_rec])
            P.dma("sp", REC[t0:t0 + 128, :], rec, reads=[r_rec], writes=[r_REC])

    for mi, (s0, n, is_ctx) in enumerate(macros):
        if mi == 0:
            make_mod_tiles(sel1)
            if debug:
                P.dma("sp", dbg["d_mod"], modrow, reads=[r_mod], writes=[r_dbg])
                P.dma("sp", dbg["d_weff"], weff, reads=[r_weff], writes=[r_dbg])
                P.dma("sp", dbg["d_shb"], shb, reads=[r_shb], writes=[r_dbg])
        if mi == 1:
            make_mod_tiles(sel0)
        nsub = n // 128
        X = XR[mi % 2]; rX = r_XR[mi % 2]
        for sub in range(nsub):
            t0 = s0 + sub * 128
            kt = t0 // 128
            xb_ = xt[nsub_done % 2]; rx = r_xt[nsub_done % 2]
            src = xrow(t0)
            P.dma("sp", xb_, src, writes=[rx])
            P.op("dve", "scalar_tensor_tensor", junk, xb_, 1.0, xb_, ALU.mult, ALU.mult, accum_out=ss1,
                 reads=[rx], writes=[r_ss1, r_junk])
            emit_rstd(P, ss1, r_ss1, rs1, r_rs1, 1.0 / D, epst, r_eps)
            P.op("dve", "scalar_tensor_tensor", htmp, xb_, rs1, weff, ALU.mult, ALU.mult,
                 reads=[rx, r_rs1, r_weff], writes=[r_htmp])
            P.op("pool", "tensor_tensor", hb, htmp, shb, ALU.add, reads=[r_htmp, r_shb], writes=[r_hb])
            if debug:
                P.op("pool", "tensor_copy", junk, hb, reads=[r_hb], writes=[r_junk])
                P.dma("sp", dbg["d_h"][t0:t0 + 128, :], junk, reads=[r_junk], writes=[r_dbg])
            pb = bankb(nsub_done % 2, 1024); rp = r_pT[nsub_done % 2]
            for kc in range(8):
                P.op("pe", "transpose", pb[:, kc * 128:(kc + 1) * 128], hb[:, kc * 128:(kc + 1) * 128], identb,
                     reads=[r_hb, r_identb], writes=[rp], inc=(kc == 7))
            P.op("act", "activation", hT[:, :, sub * 128:(sub + 1) * 128], pb.rearrange("p (a b) -> p a b", a=8), AF.Copy,
                 reads=[rp], writes=[r_hT])
            for g, (c0, c1) in enumerate(((0, 512), (512, 656))):
                pbm = PS[:, (2 + g) * 512:(2 + g) * 512 + (c1 - c0)]
                for kc in range(8):
                    P.op("pe", "matmul", pbm, hT[:, kc, sub * 128:(sub + 1) * 128], Wtm[:, kc, c0:c1],
                         start=(kc == 0), stop=(kc == 7), reads=[r_hT, r_Wtm], writes=[r_ptm], inc=(kc == 7 and g == 1))
            P.op("act", "activation", tms[:, 0:512], bank(2), AF.Copy, reads=[r_ptm], writes=[r_tms])
            P.op("act", "activation", tms[:, 512:656], bank(3, 144), AF.Copy, reads=[r_ptm], writes=[r_tms])
            if debug:
                P.dma("sp", dbg["d_tm"][t0:t0 + 128, :], tms, reads=[r_tms], writes=[r_dbg])
            P.dma("sp", Zd[t0:t0 + 128, :], tms[:, 384:640], reads=[r_tms], writes=[r_Zd])
            P.op("dve", "tensor_tensor", gt[:, 0:8], tms[:, 640:648], gp[:, 0:8], ALU.add, reads=[r_tms, r_gp], writes=[r_gt])
            P.op("act", "activation", gt[:, 0:8], gt[:, 0:8], AF.Exp, reads=[r_gt], writes=[r_gt])
            P.op("act", "activation", gt[:, 0:8], gt[:, 0:8], AF.Ln, bias=1.0, reads=[r_gt], writes=[r_gt])
            P.op("dve", "tensor_tensor", GB[:, kt, 0:8], gt[:, 0:8], gp[:, 8:16], ALU.mult, reads=[r_gt, r_gp], writes=[r_GB])
            P.op("act", "activation", gt[:, 8:16], tms[:, 648:656], AF.Exp, scale=-1.0, reads=[r_tms], writes=[r_gt])
            P.op("dve", "tensor_scalar", gt[:, 8:16], gt[:, 8:16], 1.0, None, ALU.add, reads=[r_gt], writes=[r_gt])
            P.op("dve", "reciprocal", GB[:, kt, 8:16], gt[:, 8:16], reads=[r_gt], writes=[r_GB])
            qk3 = tms[:, 0:320].rearrange("p (a b) -> p a b", a=5)
            P.op("dve", "tensor_tensor", sq5, qk3, qk3, ALU.mult, reads=[r_tms], writes=[r_sq5])
            P.op("dve", "tensor_reduce", ss5, sq5, AX.X, ALU.add, reads=[r_sq5], writes=[r_ss5])
            emit_rstd(P, ss5, r_ss5, rs5, r_rs5, 1.0 / 64, epst, r_eps)
            P.op("dve", "tensor_scalar", rs5[:, 0:4], rs5[:, 0:4], 0.125, None, ALU.mult, reads=[r_rs5], writes=[r_rs5])
            P.op("dve", "tensor_tensor", qn, qk3, rs5.unsqueeze(2).to_broadcast([128, 5, 64]), ALU.mult,
                 reads=[r_tms, r_rs5], writes=[r_qn])
            if is_ctx:
                P.op("dve", "tensor_tensor", qr[:, 0:5, :], qn, qkw, ALU.mult, reads=[r_qn, r_qkw], writes=[r_qr])
            else:
                P.op("dve", "tensor_tensor", qn, qn, qkw, ALU.mult, reads=[r_qn, r_qkw], writes=[r_qn])
                tl = t0 - NCTX
                P.dma("sp", cst[:, 0, :], dr["cos"][tl:tl + 128, :], writes=[r_cst])
                P.dma("sp", cst[:, 1, :], dr["sin"][tl:tl + 128, :], writes=[r_cst])
                cosb = cst[:, 0:1, :].to_broadcast([128, 5, 32])
                sinb = cst[:, 1:2, :].to_broadcast([128, 5, 32])
                x1 = qn[:, :, 0:32]; x2 = qn[:, :, 32:64]
                P.op("dve", "tensor_tensor", tt[0], x1, cosb, ALU.mult, reads=[r_qn, r_cst], writes=[r_tt])
                P.op("pool", "tensor_tensor", tt[1], x2, sinb, ALU.mult, reads=[r_qn, r_cst], writes=[r_tt])
                P.op("dve", "tensor_tensor", tt[2], x2, cosb, ALU.mult, reads=[r_qn, r_cst], writes=[r_tt])
                P.op("pool", "tensor_tensor", tt[3], x1, sinb, ALU.mult, reads=[r_qn, r_cst], writes=[r_tt])
                P.op("dve", "tensor_tensor", qr[:, 0:5, 0:32], tt[0], tt[1], ALU.subtract, reads=[r_tt], writes=[r_qr])
                P.op("dve", "tensor_tensor", qr[:, 0:5, 32:64], tt[2], tt[3], ALU.add, reads=[r_tt], writes=[r_qr])
            P.op("pool", "tensor_copy", qr[:, 5, :], qr[:, 4, :], reads=[r_qr], writes=[r_qr])
            P.op("pool", "tensor_copy", VA[:, kt, 0:64], tms[:, 320:384], reads=[r_tms], writes=[r_VA])
            pa = bankb(6, 384)
            qr2 = qr.rearrange("p a b -> p (a b)")
            for i in range(3):
                P.op("pe", "transpose", pa[:, i * 128:(i + 1) * 128], qr2[:, i * 128:(i + 1) * 128], identb,
                     reads=[r_qr, r_identb], writes=[r_pat], inc=(i == 2))
            if is_ctx:
                P.op("dve", "tensor_copy", QTc[:, :, t0:t0 + 128], pa[:, 0:256].rearrange("p (a b) -> p a b", a=2),
                     reads=[r_pat], writes=[r_QTc])
            else:
                tl = t0 - NCTX
                P.op("dve", "tensor_copy", QT[:, :, tl:tl + 128], pa[:, 0:256].rearrange("p (a b) -> p a b", a=2),
                     reads=[r_pat], writes=[r_QT])
            P.op("dve", "tensor_copy", KT[:, t0:t0 + 128], pa[:, 256:384], reads=[r_pat], writes=[r_KT])
            nsub_done += 1
        for ch in range(6):
            pbf = PS[:, (4 + ch % 2) * 512:(4 + ch % 2) * 512 + n]; rp = r_pfm[ch % 2]
            for kc in range(8):
                P.op("pe", "matmul", pbf, Wfm[:, kc, ch * 128:(ch + 1) * 128], hT[:, kc, 0:n],
                     start=(kc == 0), stop=(kc == 7), reads=[r_hT, r_Wfm], writes=[rp], inc=(kc == 7))
            P.op("act", "activation", X[:, ch, 2:2 + n], pbf, AF.Copy, reads=[rp], writes=[rX])
        prev_same = mi >= 2
        if prev_same:
            Xp = XR[(mi - 1) % 2]; rXp = r_XR[(mi - 1) % 2]
            P.op("pool", "tensor_copy", Xp[:, :, 514:516], X[:, :, 2:4], reads=[rX], writes=[rXp])
            P.op("pool", "tensor_copy", X[:, :, 0:2], Xp[:, :, 512:514], reads=[rXp], writes=[rX])
        else:
            P.op("pool", "memset", X[:, :, 0:2], 0.0, writes=[rX])
            if mi == 1:
                Xp = XR[0]; rXp = r_XR[0]
                P.op("pool", "memset", Xp[:, :, 2 + NCTX:4 + NCTX], 0.0, writes=[rXp])
        if mi >= 1:
            conv_macro(mi - 1)
    lastm = len(macros) - 1
    nl = macros[lastm][1]
    P.op("pool", "memset", XR[lastm % 2][:, :, 2 + nl:4 + nl], 0.0, writes=[r_XR[lastm % 2]])
    conv_macro(lastm)

    if debug:
        P.barrier()
        r_o = Res()
        st = A.alloc([2 * T], F32)
        P.op("dve", "tensor_copy", st, QT.rearrange("p a b -> p (a b)"), writes=[r_o])
        P.dma("sp", dbg["d_qt"].rearrange("p a b -> p (a b)"), st, reads=[r_o], writes=[r_dbg])
        st2 = A.alloc([NT], F32)
        P.op("dve", "tensor_copy", st2, KT, writes=[r_o])
        P.dma("sp", dbg["d_kt"], st2, reads=[r_o], writes=[r_dbg])
        st3 = A.alloc([nkt, 65], F32)
        P.op("dve", "tensor_copy", st3, VA, writes=[r_o])
        P.dma("sp", dbg["d_v"], st3, reads=[r_o], writes=[r_dbg])
        P.dma("sp", dbg["d_gb"], GB, reads=[r_o], writes=[r_dbg])
        st4 = A.alloc([2, NCTX], F32)
        P.op("dve", "tensor_copy", st4, QTc, writes=[r_o])
        P.dma("sp", dbg["d_qtc"], st4, reads=[r_o], writes=[r_dbg])

    P.barrier()
    A.release(m_p1)
    r_mix = Res()

    m_p2 = A.mark()
    PT = [A.alloc([1024], BF16) for _ in range(3)]; r_PT = [Res() for _ in range(3)]
    osb = A.alloc([512], F32, parts=65); r_osb = Res()
    ost = A.alloc([4, 64], F32); r_ost = Res()
    rcp = A.alloc([4], F32); r_rcp = Res()
    r_pS = [Res(), Res()]; r_pO = [Res(), Res()]; r_pTr = Res()
    nit = 0
    nq = 0

    def attn_block(q_ap, nqtok, ktiles, row0, col0):
        nonlocal nit, nq
        hp = q_ap.base_partition()
        po = PS[0:65, (4 + nq % 2) * 512:(4 + nq % 2) * 512 + nqtok]; rpo = r_pO[nq % 2]
        npair = len(ktiles) // 2
        def emit_pv(kp, it):
            pt = PT[it % 3]; rpt = r_PT[it % 3]
            for u in range(2):
                kt = ktiles[2 * kp + u]
                P.op("pe", "matmul", po, VA[:, kt, :], pt[:, u * 512:u * 512 + nqtok],
                     start=(kp == 0 and u == 0), stop=(kp == npair - 1 and u == 1),
                     reads=[rpt, r_VA], writes=[rpo], inc=(kp == npair - 1 and u == 1))

        for kp in range(npair):
            sb_ = nit % 2
            pS = PS[:, sb_ * 1024:(sb_ + 1) * 1024]; rS = r_pS[sb_]
            for u in range(2):
                kt = ktiles[2 * kp + u]
                P.op("pe", "matmul", pS[:, u * 512:u * 512 + nqtok], KT[hp:hp + 64, kt * 128:(kt + 1) * 128], q_ap,
                     start=True, stop=True, reads=[r_KT, r_QT, r_QTc], writes=[rS], inc=(u == 1))
            if kp >= 1:
                emit_pv(kp - 1, nit - 1)
            pt = PT[nit % 3]; rpt = r_PT[nit % 3]
            if nqtok == 512:
                P.op("act", "activation", pt, pS, AF.Exp, reads=[rS], writes=[rpt])
            else:
                P.op("act", "activation", pt.rearrange("p (u n) -> p u n", u=2)[:, :, 0:nqtok],
                     pS.rearrange("p (u n) -> p u n", u=2)[:, :, 0:nqtok], AF.Exp, reads=[rS], writes=[rpt])
            nit += 1
        emit_pv(npair - 1, nit - 1)
        P.op("dve", "tensor_copy", osb[:, 0:nqtok], po, reads=[rpo], writes=[r_osb])
        nsub = nqtok // 128
        ptr = PS[:, 6 * 512:6 * 512 + 4 * 65]
        for sub in range(nsub):
            P.op("pe", "transpose", ptr[:, sub * 65:(sub + 1) * 65], osb[:, sub * 128:(sub + 1) * 128], ident[0:65, 0:65],
                 reads=[r_osb, r_ident], writes=[r_pTr], inc=(sub == nsub - 1))
        ptr3 = ptr.rearrange("p (s c) -> p s c", s=4)
        P.op("dve", "reciprocal", rcp[:, 0:nsub], ptr3[:, 0:nsub, 64], reads=[r_pTr], writes=[r_rcp])
        P.op("dve", "tensor_tensor", ost[:, 0:nsub, :], ptr3[:, 0:nsub, 0:64],
             rcp[:, 0:nsub].unsqueeze(2).to_broadcast([128, nsub, 64]), ALU.mult, reads=[r_pTr, r_rcp], writes=[r_ost])
        P.dma("sp", mix[row0:row0 + nqtok, col0:col0 + 64].rearrange("(s p) c -> p s c", p=128), ost[:, 0:nsub, :],
              reads=[r_ost], writes=[r_mix])
        nq += 1

    all_kt = list(range(nkt))
    for h in range(4 if do_attn else 0):
        pair, half = h // 2, h % 2
        for qt in range(T // 512):
            attn_block(QT[half * 64:(half + 1) * 64, pair, qt * 512:(qt + 1) * 512], 512, all_kt,
                       NCTX + qt * 512, 256 + h * 64)
        if want_ctx_out:
            attn_block(QTc[half * 64:(half + 1) * 64, pair, :], NCTX, [0, 1], 0, 256 + h * 64)
    P.barrier()
    A.release(m_att)

    tri = A.alloc([2, 128], F32); r_tri = Res()
    bigm = A.alloc([2, 512], F32); r_bigm = Res()
    strict = A.alloc([4, 512], F32); r_strict = Res()
    P.dma("sp", tri, dr["tri"].rearrange("p (a b) -> p a b", a=2), writes=[r_tri])
    P.dma("sp", bigm, dr["bigm"].rearrange("p (a b) -> p a b", a=2), writes=[r_bigm])
    P.dma("sp", strict, dr["strict"].rearrange("p (a b) -> p a b", a=4), writes=[r_strict])
    blk = A.alloc([128], F32); r_blk = Res()
    P.op("dve", "memset", blk, 0.0, writes=[r_blk])
    P.op("dve", "memset", blk[0:64, 0:64], 1.0, writes=[r_blk])
    P.op("dve", "memset", blk[64:128, 64:128], 1.0, writes=[r_blk])
    Oacc = A.alloc([nkt, 256], F32); r_Oacc = [Res() for _ in range(nkt)]
    written = [False] * nkt

    class DS:
        pass
    st = []
    for d_ in range(2):
        z = DS()
        z.R = A.alloc([768], F32); z.r_R = Res()
        z.Kb = A.alloc([4, 64], F32); z.r_Kb = Res()
        z.Qd = A.alloc([4, 64], F32); z.r_Qd = Res()
        z.kd = A.alloc([4, 64], F32); z.r_kd = Res()
        z.G = A.alloc([4], F32); z.r_G = Res()
        z.nG = A.alloc([4], F32); z.r_nG = Res()
        z.eG = A.alloc([4], F32); z.r_eG = Res()
        z.dG = A.alloc([4], F32); z.r_dG = Res()
        z.GL = A.alloc([2], F32); z.r_GL = Res()
        z.TT = A.alloc([8, 128], F32); z.r_TT = Res()
        z.KbP = A.alloc([2, 2, 128], F32); z.r_KbP = Res()
        z.QnP = A.alloc([2, 2, 128], F32); z.r_QnP = Res()
        P.op("pool", "memset", z.KbP, 0.0, writes=[z.r_KbP])
        P.op("pool", "memset", z.QnP, 0.0, writes=[z.r_QnP])
        z.decT = A.alloc([4, 128], F32); z.r_decT = Res()
        z.Bm = A.alloc([4, 128], F32); z.r_Bm = Res()
        z.BmO = A.alloc([4, 128], F32); z.r_BmO = Res()
        z.atT = A.alloc([4, 128], F32); z.r_atT = Res()
        z.Bp = [A.alloc([4, 128], F32) for _ in range(6)]; z.r_Bp = [Res() for _ in range(6)]
        z.BO = A.alloc([4, 128], F32); z.r_BO = Res()
        z.Ac = [A.alloc([4, 128], F32) for _ in range(2)]; z.r_Ac = [Res(), Res()]
        z.r0 = A.alloc([4, 128], F32); z.r_r0 = Res()
        z.r = A.alloc([4, 128], F32); z.r_r = Res()
        z.wc = A.alloc([4, 64], F32); z.r_wc = Res()
        z.wT = A.alloc([2, 128], F32); z.r_wT = Res()
        z.vn = A.alloc([4, 64], F32); z.r_vn = Res()
        z.tS = A.alloc([2, 128], F32); z.r_tS = Res()
        z.S = A.alloc([2, 128], F32); z.r_S = Res()
        P.op("pool", "memset", z.S, 0.0, writes=[z.r_S])
        st.append(z)
    r_b = [Res() for _ in range(8)]

    def dn_visit(c, dlt):
        z = st[dlt]
        last = 127 if dlt == 0 else 0
        t0 = c * 128
        P.dma("sp", z.R, REC[t0:t0 + 128, :], reads=[r_REC], writes=[z.r_R])
        g4 = GB[:, c, dlt * 4:dlt * 4 + 4]
        be4 = GB[:, c, 8 + dlt * 4:8 + dlt * 4 + 4]
        Qn3 = z.R[:, 0:256].rearrange("p (a b) -> p a b", a=4)
        Kn3 = z.R[:, 256:512].rearrange("p (a b) -> p a b", a=4)
        V3 = z.R[:, 512:768].rearrange("p (a b) -> p a b", a=4)
        r3 = z.r
        r03 = z.r0
        bs = 4 * dlt
        pD = PS[:, bs * 512:(bs + 1) * 512]; rD = r_b[bs]
        for h in range(4):
            P.op("pe", "matmul", pD[:, h * 128:(h + 1) * 128], g4[:, h:h + 1].to_broadcast([128, 128]), tri[:, dlt, :],
                 start=True, stop=False, reads=[r_GB, r_tri], writes=[rD], inc=False)
            P.op("pe", "matmul", pD[:, h * 128:(h + 1) * 128], ident, bigm[:, dlt, 0:128], start=False, stop=True,
                 reads=[r_ident, r_bigm], writes=[rD], inc=(h == 3))
        pG = PS[:, (bs + 3) * 512:(bs + 3) * 512 + 4]; rG = r_b[bs + 3]
        P.op("pe", "matmul", pG, tri[:, dlt, :], g4, start=True, stop=True, reads=[r_GB, r_tri], writes=[rG])
        P.op("dve", "tensor_copy", z.G, pG, reads=[rG], writes=[z.r_G])
        P.op("dve", "tensor_scalar", z.nG, z.G, -1.0, None, ALU.mult, reads=[z.r_G], writes=[z.r_nG])
        P.op("act", "activation", z.eG, z.G, AF.Exp, reads=[z.r_G], writes=[z.r_eG])
        pDl = pD.rearrange("p (h i) -> p h i", h=4)[:, :, last]
        P.op("dve", "tensor_tensor", z.dG, pDl, z.G, ALU.subtract, reads=[rD, z.r_G], writes=[z.r_dG])
        P.op("act", "activation", z.dG, z.dG, AF.Exp, reads=[z.r_dG], writes=[z.r_dG])
        pD4 = pD.rearrange("p (a b i) -> p a b i", a=2, b=2)
        P.op("act", "activation", z.GL[0:64, :], pD4[0:64, :, 0, last], AF.Exp, reads=[rD], writes=[z.r_GL])
        P.op("act", "activation", z.GL[64:128, :], pD4[64:128, :, 1, last], AF.Exp, reads=[rD], writes=[z.r_GL])
        for h in range(4):
            P.op("act", "activation", z.decT[:, h, :], pD[:, h * 128:(h + 1) * 128], AF.Exp, bias=z.nG[:, h:h + 1],
                 reads=[rD, z.r_nG], writes=[z.r_decT])
        if dn_stage < 1:
            return
        yield
        P.op("pool", "tensor_tensor", z.Kb, Kn3, be4.unsqueeze(2).to_broadcast([128, 4, 64]), ALU.mult,
             reads=[z.r_R, r_GB], writes=[z.r_Kb])
        P.op("pool", "tensor_tensor", r03[:, :, 0:64], V3, be4.unsqueeze(2).to_broadcast([128, 4, 64]), ALU.mult,
             reads=[z.r_R, r_GB], writes=[z.r_r0])
        P.op("pool", "tensor_tensor", r03[:, :, 64:128], z.Kb, z.eG.unsqueeze(2).to_broadcast([128, 4, 64]), ALU.mult,
             reads=[z.r_Kb, z.r_eG], writes=[z.r_r0])
        P.op("pool", "tensor_tensor", z.Qd, Qn3, z.eG.unsqueeze(2).to_broadcast([128, 4, 64]), ALU.mult,
             reads=[z.r_R, z.r_eG], writes=[z.r_Qd])
        P.op("pool", "tensor_tensor", z.kd, Kn3, z.dG.unsqueeze(2).to_broadcast([128, 4, 64]), ALU.mult,
             reads=[z.r_R, z.r_dG], writes=[z.r_kd])
        if dn_stage < 2:
            return
        yield
        pT = PS[:, (bs + 1) * 512:(bs + 3) * 512]; rT = r_b[bs + 1]; rT2 = r_b[bs + 2]
        Kb2 = z.Kb.rearrange("p a b -> p (a b)"); Qd2 = z.Qd.rearrange("p a b -> p (a b)")
        srcs = [(z.R[:, 256:384], z.r_R), (z.R[:, 384:512], z.r_R), (Kb2[:, 0:128], z.r_Kb), (Kb2[:, 128:256], z.r_Kb),
                (z.R[:, 0:128], z.r_R), (z.R[:, 128:256], z.r_R), (Qd2[:, 0:128], z.r_Qd), (Qd2[:, 128:256], z.r_Qd)]
        for i, (sap, sr) in enumerate(srcs):
            P.op("pe", "transpose", pT[:, i * 128:(i + 1) * 128], sap, ident, reads=[sr, r_ident], writes=[rT, rT2], inc=(i == 7))
        pT3 = pT.rearrange("p (a b) -> p a b", a=8)
        P.op("act", "activation", z.TT[:, 0:2, :], pT3[:, 0:2, :], AF.Copy, reads=[rT, rT2], writes=[z.r_TT])
        P.op("dve", "tensor_copy", z.TT[:, 6:8, :], pT3[:, 6:8, :], reads=[rT, rT2], writes=[z.r_TT])
        for half in range(2):
            lo, hi = half * 64, half * 64 + 64
            P.op("act", "activation", z.KbP[lo:hi, :, half, :], pT3[lo:hi, 2:4, :], AF.Copy, reads=[rT, rT2], writes=[z.r_KbP])
            P.op("dve", "tensor_copy", z.QnP[lo:hi, :, half, :], pT3[lo:hi, 4:6, :], reads=[rT, rT2], writes=[z.r_QnP])
        if dn_stage < 2.3:
            return
        yield
        pKK = PS[:, (bs + 1) * 512:(bs + 2) * 512]; rKK = r_b[bs + 1]
        pKQ = PS[:, (bs + 2) * 512:(bs + 3) * 512]; rKQ = r_b[bs + 2]
        for h in range(4):
            pair, half = h // 2, h % 2
            P.op("pe", "matmul", pKK[:, h * 128:(h + 1) * 128], z.TT[:, 0 + pair, :], z.KbP[:, pair, half, :],
                 start=True, stop=True, reads=[z.r_TT, z.r_KbP], writes=[rKK], inc=(h == 3))
        for h in range(4):
            pair, half = h // 2, h % 2
            P.op("pe", "matmul", pKQ[:, h * 128:(h + 1) * 128], z.TT[:, 0 + pair, :], z.QnP[:, pair, half, :],
                 start=True, stop=True, reads=[z.r_TT, z.r_QnP], writes=[rKQ], inc=(h == 3))
        if dn_stage < 2.6:
            return
        P.op("dve", "tensor_tensor", z.Bm.rearrange("p a b -> p (a b)"), pKK, strict[:, 2 * dlt, :], ALU.mult,
             reads=[rKK, r_strict], writes=[z.r_Bm])
        P.op("dve", "tensor_tensor", z.BmO.rearrange("p a b -> p (a b)"), pKK, strict[:, 2 * dlt + 1, :], ALU.mult,
             reads=[rKK, r_strict], writes=[z.r_BmO])
        P.op("pool", "tensor_tensor", z.Bp[0], z.Bm, z.decT, ALU.mult, reads=[z.r_Bm, z.r_decT], writes=[z.r_Bp[0]])
        P.op("pool", "tensor_tensor", z.BO, z.BmO, z.decT, ALU.mult, reads=[z.r_BmO, z.r_decT], writes=[z.r_BO])
        P.op("dve", "tensor_tensor", z.atT.rearrange("p a b -> p (a b)"), pKQ, z.decT.rearrange("p a b -> p (a b)"), ALU.mult,
             reads=[rKQ, z.r_decT], writes=[z.r_atT])
        if dn_stage < 3:
            return
        yield
        pA = PS[:, (bs + 3) * 512:(bs + 4) * 512]; rA = r_b[bs + 3]
        pB = PS[:, (bs + 2) * 512:(bs + 3) * 512]; rB = r_b[bs + 2]
        for h in range(4):
            P.op("pe", "transpose", pA[:, h * 128:(h + 1) * 128], z.Bp[0][:, h, :], ident,
                 reads=[z.r_Bp[0], r_ident], writes=[rA], inc=(h == 3))
        P.op("act", "activation", z.Ac[0].rearrange("p a b -> p (a b)"), pA, AF.Copy, reads=[rA], writes=[z.r_Ac[0]])
        yield
        for k in range(5):
            cur, nxt = k % 2, (k + 1) % 2
            for h in range(4):
                P.op("pe", "matmul", pB[:, h * 128:(h + 1) * 128], z.Ac[cur][:, h, :], z.Bp[k][:, h, :],
                     start=True, stop=True, reads=[z.r_Ac[cur], z.r_Bp[k]], writes=[rB], inc=(h == 3))
            P.op("act", "activation", z.Bp[k + 1].rearrange("p a b -> p (a b)"), pB, AF.Copy, reads=[rB], writes=[z.r_Bp[k + 1]])
            if k < 4:
                for h in range(4):
                    P.op("pe", "matmul", pA[:, h * 128:(h + 1) * 128], z.Bp[k][:, h, :], z.Ac[cur][:, h, :],
                         start=True, stop=True, reads=[z.r_Ac[cur], z.r_Bp[k]], writes=[rA], inc=(h == 3))
                P.op("act", "activation", z.Ac[nxt].rearrange("p a b -> p (a b)"), pA, AF.Copy, reads=[rA], writes=[z.r_Ac[nxt]])
            yield
        yield
        pR = pKK; rR = rKK
        r2 = r3.rearrange("p a b -> p (a b)")
        r02 = r03.rearrange("p a b -> p (a b)")
        for sweep in range(2):
            for k in range(6):
                src = r03 if (sweep == 0 and k == 0) else r3
                rsrc = z.r_r0 if (sweep == 0 and k == 0) else z.r_r
                for h in range(4):
                    P.op("pe", "matmul", pR[:, h * 128:(h + 1) * 128], z.Bp[k][:, h, :], src[:, h, :], start=True, stop=True,
                         reads=[z.r_Bp[k], rsrc], writes=[rR], inc=(h == 3))
                P.op("dve", "tensor_tensor", r2, src.rearrange("p a b -> p (a b)"), pR,
                     ALU.subtract if k == 0 else ALU.add, reads=[rsrc, rR], writes=[z.r_r])
                yield
            if sweep == 0:
                for h in range(4):
                    P.op("pe", "matmul", pR[:, h * 128:(h + 1) * 128], z.BO[:, h, :], r3[:, h, :], start=True, stop=True,
                         reads=[z.r_BO, z.r_r], writes=[rR], inc=(h == 3))
                P.op("dve", "tensor_tensor", r2, r02, pR, ALU.subtract, reads=[z.r_r0, rR], writes=[z.r_r])
        if dn_stage < 4:
            return
        yield
        P.op("pool", "tensor_copy", z.wc, r3[:, :, 64:128], reads=[z.r_r], writes=[z.r_wc])
        wc2 = z.wc.rearrange("p a b -> p (a b)")
        pW = PS[:, (bs + 3) * 512 + 256:(bs + 4) * 512]; rW = r_b[bs + 3]
        for pr in range(2):
            P.op("pe", "transpose", pW[:, pr * 128:(pr + 1) * 128], wc2[:, pr * 128:(pr + 1) * 128], ident,
                 reads=[z.r_wc, r_ident], writes=[rW], inc=(pr == 1))
        P.op("act", "activation", z.wT.rearrange("p a b -> p (a b)"), pW, AF.Copy, reads=[rW], writes=[z.r_wT])
        if dn_stage < 5:
            return
        yield
        pWS = PS[:, (bs + 2) * 512:(bs + 2) * 512 + 256]; pDS = PS[:, (bs + 2) * 512 + 256:(bs + 3) * 512]; rWS = r_b[bs + 2]
        pO = PS[:, (bs + 3) * 512:(bs + 3) * 512 + 256]
        for pr in range(2):
            P.op("pe", "matmul", pWS[:, pr * 128:(pr + 1) * 128], z.wT[:, pr, :], z.S[:, pr, :], start=True, stop=True,
                 reads=[z.r_wT, z.r_S], writes=[rWS], inc=(pr == 1))
        P.op("dve", "tensor_tensor", z.vn, r3[:, :, 0:64], pWS.rearrange("p (a b) -> p a b", a=4), ALU.subtract,
             reads=[z.r_r, rWS], writes=[z.r_vn])
        vn2 = z.vn.rearrange("p a b -> p (a b)")
        kd2 = z.kd.rearrange("p a b -> p (a b)")
        for pr in range(2):
            P.op("pe", "matmul", pO[:, pr * 128:(pr + 1) * 128], z.TT[:, 6 + pr, :], z.S[:, pr, :], start=True, stop=False,
                 reads=[z.r_TT, z.r_S], writes=[rW], inc=False)
            for hf in range(2):
                h = 2 * pr + hf
                P.op("pe", "matmul", pO[:, h * 64:(h + 1) * 64], z.atT[:, h, :], z.vn[:, h, :], start=False, stop=(hf == 1),
                     reads=[z.r_atT, z.r_vn], writes=[rW], inc=(hf == 1))
        for pr in range(2):
            P.op("pe", "matmul", pDS[:, pr * 128:(pr + 1) * 128], kd2[:, pr * 128:(pr + 1) * 128], vn2[:, pr * 128:(pr + 1) * 128],
                 start=True, stop=True, reads=[z.r_kd, z.r_vn], writes=[rWS], inc=(pr == 1))
        yield
        if not written[c]:
            P.op("act", "activation", Oacc[:, c, :], pO, AF.Copy, reads=[rW], writes=[r_Oacc[c]])
            written[c] = True
        else:
            P.op("dve", "tensor_tensor", Oacc[:, c, :], Oacc[:, c, :], pO, ALU.add, reads=[rW, r_Oacc[c]], writes=[r_Oacc[c]])
        P.op("dve", "tensor_tensor", z.tS, pDS.rearrange("p (a b) -> p a b", a=2), blk.unsqueeze(1).to_broadcast([128, 2, 128]),
             ALU.mult, reads=[rWS, r_blk], writes=[z.r_tS])
        for pr in range(2):
            P.op("dve", "scalar_tensor_tensor", z.S[:, pr, :], z.S[:, pr, :], z.GL[:, pr:pr + 1], z.tS[:, pr, :], ALU.mult, ALU.add,
                 reads=[z.r_S, z.r_GL, z.r_tS], writes=[z.r_S])

    fwd_order = list(range(nkt))
    bwd_order = [1, 0] + list(range(nkt - 1, 1, -1))
    for s_ in range(nkt if do_dn else 0):
        gens = [dn_visit(fwd_order[s_], 0), dn_visit(bwd_order[s_], 1)]
        while gens:
            for g_ in list(gens):
                try:
                    next(g_)
                except StopIteration:
                    gens.remove(g_)

    dnw = A.alloc([256], F32); r_dnw = Res()
    P.dma("sp", dnw, dr["dnw"], writes=[r_dnw])
    zt = [A.alloc([256], F32) for _ in range(2)]; r_zt = [Res(), Res()]
    osq = A.alloc([256], F32); r_osq = Res()
    os4 = A.alloc([4], F32); r_os4 = Res()
    or4 = A.alloc([4], F32); r_or4 = Res()
    om = [A.alloc([256], F32) for _ in range(2)]; r_om = [Res(), Res()]
    c_start = 0 if want_ctx_out else 2
    for c in range(c_start, nkt if do_dn else 0):
        i2 = c % 2
        P.dma("sp", zt[i2], Zd[c * 128:(c + 1) * 128, :], reads=[r_Zd], writes=[r_zt[i2]])
        P.op("act", "activation", zt[i2], zt[i2], AF.Silu, reads=[r_zt[i2]], writes=[r_zt[i2]])
        P.op("dve", "tensor_tensor", osq, Oacc[:, c, :], Oacc[:, c, :], ALU.mult, reads=[r_Oacc[c]], writes=[r_osq])
        P.op("dve", "tensor_reduce", os4, osq.rearrange("p (a b) -> p a b", a=4), AX.X, ALU.add, reads=[r_osq], writes=[r_os4])
        emit_rstd(P, os4, r_os4, or4, r_or4, 1.0 / 64, epst, r_eps)
        P.op("dve", "tensor_tensor", om[i2].rearrange("p (a b) -> p a b", a=4), Oacc[:, c, :].rearrange("p (a b) -> p a b", a=4),
             or4.unsqueeze(2).to_broadcast([128, 4, 64]), ALU.mult, reads=[r_Oacc[c], r_or4], writes=[r_om[i2]])
        P.op("dve", "tensor_tensor", om[i2], om[i2], dnw, ALU.mult, reads=[r_om[i2], r_dnw], writes=[r_om[i2]])
        P.op("dve", "tensor_tensor", om[i2], om[i2], zt[i2], ALU.mult, reads=[r_om[i2], r_zt[i2]], writes=[r_om[i2]])
        P.dma("sp", mix[c * 128:(c + 1) * 128, 0:256], om[i2], reads=[r_om[i2]], writes=[r_mix])

    P.barrier()
    print("stage M emitted: instructions", P.ninst, "arena peak", A.peak)


def emit_F(P, A, PS, dr, blocks, moe, final_norm, F, NE, xrow, mixG, NT, ZP):
    P.barrier()
    A.release(0)

    def bank(i, n=512):
        return PS[:, i * 512:i * 512 + n]

    def bankb(i, n=1024):
        return PS[:, i * 512:(i + 1) * 512].bitcast(BF16)[:, 0:n]

    ident = A.alloc([128], F32); r_ident = Res()
    identb = A.alloc([128], BF16); r_identb = Res()
    P.dma("sp", ident, dr["ident"], writes=[r_ident])
    P.dma("pool", identb, dr["ident"], writes=[r_identb])
    epst = A.alloc([1], F32); r_eps = Res()
    P.op("dve", "memset", epst, EPS, writes=[r_eps])
    sel0, sel1, r_sel = emit_sel(P, A)
    g1b = A.alloc([D], F32); r_g1b = Res()
    weff = A.alloc([D], F32); r_weff = Res()
    shb = A.alloc([D], F32); r_shb = Res()
    g2b = A.alloc([D], F32); r_g2b = Res()
    r_psb = Res()

    def make_mod_tiles(sel):
        mm = A.mark()
        modrow, r_mod = emit_mod(P, A, PS, dr, 4, None)
        nwrow = A.alloc([D], F32, parts=2); r_nw = Res()
        P.dma("sp", nwrow, dr["nw"], writes=[r_nw])
        emit_bcast(P, PS, r_psb, sel, r_sel, modrow[:, 0:1024], r_mod, g1b, r_g1b, bank=7)
        emit_bcast(P, PS, r_psb, sel, r_sel, modrow[:, 2048:3072], r_mod, weff, r_weff, bank=7)
        P.op("dve", "tensor_scalar", weff, weff, 1.0, None, ALU.add, reads=[r_weff], writes=[r_weff])
        emit_bcast(P, PS, r_psb, sel, r_sel, nwrow, r_nw, shb, r_shb, bank=7)
        P.op("dve", "tensor_tensor", weff, weff, shb, ALU.mult, reads=[r_weff, r_shb], writes=[r_weff])
        emit_bcast(P, PS, r_psb, sel, r_sel, modrow[:, 1024:2048], r_mod, shb, r_shb, bank=7)
        emit_bcast(P, PS, r_psb, sel, r_sel, modrow[:, 3072:4096], r_mod, g2b, r_g2b, bank=7)
        P.barrier()
        A.release(mm)

    NBmax = max(b[1] for b in blocks) // 128
    X1 = A.alloc([NBmax, D], F32); r_X1 = [Res() for _ in range(NBmax)]
    h2T = A.alloc([8, NBmax * 128], BF16); r_h2T = Res()
    RW = A.alloc([NBmax, 8], F32); r_RW = Res()
    ss1 = A.alloc([1], F32); r_ss1 = Res()
    rs1 = A.alloc([1], F32); r_rs1 = Res()
    rbt = A.alloc([8], F32); r_rbt = Res()
    P.dma("sp", rbt, dr["rb"], writes=[r_rbt])
    m_ph = A.mark()
    r_out = Res()
    r_bk = [Res() for _ in range(8)]
    cur_sel = None
    for (row0, ntok, is_ctx) in blocks:
        NB = ntok // 128
        sel = sel1 if is_ctx else sel0
        A.release(m_ph)
        if cur_sel is not sel:
            make_mod_tiles(sel)
            cur_sel = sel
        Wout = A.alloc([8, D], BF16); r_Wout = Res()
        P.dma("pool", Wout, dr["wout"].rearrange("(c k) n -> k c n", k=128), writes=[r_Wout])
        Wr = A.alloc([8, 8], F32); r_Wr = Res()
        P.dma("sp", Wr, dr["wr"].rearrange("(c k) n -> k c n", k=128), writes=[r_Wr])
        mb = [A.alloc([D], BF16) for _ in range(2)]; r_mb = [Res(), Res()]
        xt = [A.alloc([D], F32) for _ in range(2)]; r_xt = [Res(), Res()]
        mixT = A.alloc([8, 128], BF16); r_mixT = Res()
        tmpf = A.alloc([D], F32); r_tmpf = Res()
        h2f = A.alloc([D], F32); r_h2f = Res()
        h2b = A.alloc([D], BF16); r_h2b = Res()
        h2Tf = A.alloc([8, 128], F32); r_h2Tf = Res()
        junk = A.alloc([D], F32); r_junk = Res()
        lg = A.alloc([8], F32); r_lg = Res()
        l2 = A.alloc([8], F32); r_l2 = Res()
        eq1 = A.alloc([8], F32); r_eq1 = Res()
        eq2 = A.alloc([8], F32); r_eq2 = Res()
        sm = A.alloc([8], F32); r_sm = Res()
        for s in range(NB):
            r0 = row0 + s * 128
            i2 = s % 2
            P.dma("pool", mb[i2][:, 0:512], mixG[r0:r0 + 128, :], writes=[r_mb[i2]])
            P.dma("pool", mb[i2][:, 512:1024], mixG[NT + r0:NT + r0 + 128, :], writes=[r_mb[i2]])
            P.dma("sp", xt[i2], xrow(r0), writes=[r_xt[i2]])
            pb = bankb(6, 1024)
            for kc in range(8):
                P.op("pe", "transpose", pb[:, kc * 128:(kc + 1) * 128], mb[i2][:, kc * 128:(kc + 1) * 128], identb,
                     reads=[r_mb[i2], r_identb], writes=[r_bk[6]], inc=(kc == 7))
            P.op("act", "activation", mixT, pb.rearrange("p (a b) -> p a b", a=8), AF.Copy, reads=[r_bk[6]], writes=[r_mixT])
            for half in range(2):
                py = bank(4 + half)
                for kc in range(8):
                    P.op("pe", "matmul", py, mixT[:, kc, :], Wout[:, kc, half * 512:(half + 1) * 512],
                         start=(kc == 0), stop=(kc == 7), reads=[r_mixT, r_Wout], writes=[r_bk[4 + half]], inc=(kc == 7))
                hs = slice(half * 512, (half + 1) * 512)
                P.op("dve", "tensor_tensor", tmpf[:, hs], py, g1b[:, hs], ALU.mult, reads=[r_bk[4 + half], r_g1b], writes=[r_tmpf])
                P.op("pool", "tensor_tensor", X1[:, s, hs], tmpf[:, hs], xt[i2][:, hs], ALU.add,
                     reads=[r_tmpf, r_xt[i2]], writes=[r_X1[s]])
            P.op("dve", "scalar_tensor_tensor", junk, X1[:, s, :], 1.0, X1[:, s, :], ALU.mult, ALU.mult, accum_out=ss1,
                 reads=[r_X1[s]], writes=[r_ss1, r_junk])
            emit_rstd(P, ss1, r_ss1, rs1, r_rs1, 1.0 / D, epst, r_eps)
            P.op("dve", "scalar_tensor_tensor", tmpf, X1[:, s, :], rs1, weff, ALU.mult, ALU.mult,
                 reads=[r_X1[s], r_rs1, r_weff], writes=[r_tmpf])
            if moe:
                P.op("pool", "tensor_tensor", h2f, tmpf, shb, ALU.add, reads=[r_tmpf, r_shb], writes=[r_h2f])
                for g in range(2):
                    pt = PS[:, (6 + g) * 512:(7 + g) * 512]
                    for kc in range(4):
                        P.op("pe", "transpose", pt[:, kc * 128:(kc + 1) * 128], h2f[:, (4 * g + kc) * 128:(4 * g + kc + 1) * 128], ident,
                             reads=[r_h2f, r_ident], writes=[r_bk[6 + g]], inc=(kc == 3))
                    P.op("act", "activation", h2Tf[:, 4 * g:4 * g + 4, :], pt.rearrange("p (a b) -> p a b", a=4), AF.Copy,
                         reads=[r_bk[6 + g]], writes=[r_h2Tf])
                P.op("pool", "tensor_copy", h2T[:, :, s * 128:(s + 1) * 128], h2Tf, reads=[r_h2Tf], writes=[r_h2T])
                pl = PS[:, 4 * 512:4 * 512 + 8]
                for kc in range(8):
                    P.op("pe", "matmul", pl, h2Tf[:, kc, :], Wr[:, kc, :], start=(kc == 0), stop=(kc == 7),
                         reads=[r_h2Tf, r_Wr], writes=[r_bk[4]], inc=(kc == 7))
                P.op("dve", "tensor_tensor", lg, pl, rbt, ALU.add, reads=[r_bk[4], r_rbt], writes=[r_lg])
                P.op("dve", "tensor_reduce", sm[:, 0:1], lg, AX.X, ALU.max, reads=[r_lg], writes=[r_sm])
                P.op("dve", "tensor_scalar", eq1, lg, sm[:, 0:1], None, ALU.is_equal, reads=[r_lg, r_sm], writes=[r_eq1])
                P.op("dve", "scalar_tensor_tensor", l2, eq1, -1e30, lg, ALU.mult, ALU.add, reads=[r_eq1, r_lg], writes=[r_l2])
                P.op("dve", "tensor_reduce", sm[:, 1:2], l2, AX.X, ALU.max, reads=[r_l2], writes=[r_sm])
                P.op("dve", "tensor_scalar", eq2, l2, sm[:, 1:2], None, ALU.is_equal, reads=[r_l2, r_sm], writes=[r_eq2])
                P.op("dve", "tensor_tensor", sm[:, 2:3], sm[:, 1:2], sm[:, 0:1], ALU.subtract, reads=[r_sm], writes=[r_sm])
                P.op("act", "activation", sm[:, 3:4], sm[:, 2:3], AF.Exp, reads=[r_sm], writes=[r_sm])
                P.op("dve", "tensor_scalar", sm[:, 4:5], sm[:, 3:4], 1.0, None, ALU.add, reads=[r_sm], writes=[r_sm])
                P.op("dve", "reciprocal", sm[:, 5:6], sm[:, 4:5], reads=[r_sm], writes=[r_sm])
                P.op("dve", "tensor_tensor", sm[:, 6:7], sm[:, 3:4], sm[:, 5:6], ALU.mult, reads=[r_sm], writes=[r_sm])
                P.op("dve", "tensor_scalar", RW[:, s, :], eq1, sm[:, 5:6], None, ALU.mult, reads=[r_eq1, r_sm], writes=[r_RW])
                P.op("dve", "scalar_tensor_tensor", RW[:, s, :], eq2, sm[:, 6:7], RW[:, s, :], ALU.mult, ALU.add,
                     reads=[r_eq2, r_sm, r_RW], writes=[r_RW])
            else:
                P.op("pool", "tensor_tensor", h2b, tmpf, shb, ALU.add, reads=[r_tmpf, r_shb], writes=[r_h2b])
                pt = bankb(7, 1024)
                for kc in range(8):
                    P.op("pe", "transpose", pt[:, kc * 128:(kc + 1) * 128], h2b[:, kc * 128:(kc + 1) * 128], identb,
                         reads=[r_h2b, r_identb], writes=[r_bk[7]], inc=(kc == 7))
                P.op("act", "activation", h2T[:, :, s * 128:(s + 1) * 128], pt.rearrange("p (a b) -> p a b", a=8), AF.Copy,
                     reads=[r_bk[7]], writes=[r_h2T])
        for s in range(NB):
            P.op("pool", "tensor_scalar", X1[:, s, :], X1[:, s, :], 0.5, None, ALU.mult, reads=[r_X1[s]], writes=[r_X1[s]])
        P.barrier()
        A.release(m_ph)
        GS = 4
        wg = [A.alloc([8, GS * 128], BF16) for _ in range(2)]; r_wg = [Res(), Res()]
        wu = [A.alloc([8, GS * 128], BF16) for _ in range(2)]; r_wu = [Res(), Res()]
        wd = [A.alloc([GS, D], BF16) for _ in range(2)]; r_wd = [Res(), Res()]
        sg = [A.alloc([512], F32) for _ in range(2)]; r_sg = [Res(), Res()]
        actT = [A.alloc([GS, 512], BF16) for _ in range(2)]; r_actT = [Res(), Res()]
        nchunk = F // 128
        groups = [(c0, min(GS, nchunk - c0)) for c0 in range(0, nchunk, GS)]
        macros = [(m0, min(512, ntok - m0)) for m0 in range(0, ntok, 512)]
        gi = 0
        mcount = 0
        fcount = 0
        ycount = 0
        for e in range(NE):
            for (c0, gsz) in groups:
                b = gi % 2
                f0 = c0 * 128
                P.dma("pool", wg[b][:, :, 0:gsz * 128], dr["wg"][e, :, f0:f0 + gsz * 128].rearrange("(c k) n -> k c n", k=128),
                      writes=[r_wg[b]])
                P.dma("pool", wu[b][:, :, 0:gsz * 128], dr["wu"][e, :, f0:f0 + gsz * 128].rearrange("(c k) n -> k c n", k=128),
                      writes=[r_wu[b]])
                P.dma("pool", wd[b][:, 0:gsz, :], dr["wd"][e, f0:f0 + gsz * 128, :].rearrange("(c k) n -> k c n", k=128),
                      writes=[r_wd[b]])
                P.op("pool", "tensor_tensor", wd[b][:, 0:gsz, :], wd[b][:, 0:gsz, :], g2b.unsqueeze(1).to_broadcast([128, gsz, D]),
                     ALU.mult, reads=[r_wd[b], r_g2b], writes=[r_wd[b]])
                for (m0, mn) in macros:
                    ab = mcount % 2
                    for fc in range(gsz):
                        pg = PS[:, (0 + fcount % 2) * 512:(0 + fcount % 2) * 512 + mn]; rpg = r_bk[0 + fcount % 2]
                        pu = PS[:, (2 + fcount % 2) * 512:(2 + fcount % 2) * 512 + mn]; rpu = r_bk[2 + fcount % 2]
                        for kc in range(8):
                            P.op("pe", "matmul", pg, wg[b][:, kc, fc * 128:(fc + 1) * 128], h2T[:, kc, m0:m0 + mn],
                                 start=(kc == 0), stop=(kc == 7), reads=[r_wg[b], r_h2T], writes=[rpg], inc=(kc == 7))
                        for kc in range(8):
                            P.op("pe", "matmul", pu, wu[b][:, kc, fc * 128:(fc + 1) * 128], h2T[:, kc, m0:m0 + mn],
                                 start=(kc == 0), stop=(kc == 7), reads=[r_wu[b], r_h2T], writes=[rpu], inc=(kc == 7))
                        sb_ = fcount % 2
                        P.op("act", "activation", sg[sb_][:, 0:mn], pg, AF.Silu, reads=[rpg], writes=[r_sg[sb_]])
                        P.op("dve", "tensor_tensor", actT[ab][:, fc, 0:mn], sg[sb_][:, 0:mn], pu, ALU.mult,
                             reads=[r_sg[sb_], rpu], writes=[r_actT[ab]])
                        fcount += 1
                    for sub in range(mn // 128):
                        sgl = (m0 // 128) + sub
                        for half in range(2):
                            py = PS[:, (4 + ycount % 2) * 512:(5 + ycount % 2) * 512]; rpy = r_bk[4 + ycount % 2]
                            for fc in range(gsz):
                                P.op("pe", "matmul", py, actT[ab][:, fc, sub * 128:(sub + 1) * 128], wd[b][:, fc, half * 512:(half + 1) * 512],
                                     start=(fc == 0), stop=(fc == gsz - 1), reads=[r_actT[ab], r_wd[b]], writes=[rpy], inc=(fc == gsz - 1))
                            hs = slice(half * 512, (half + 1) * 512)
                            scal = RW[:, sgl, e:e + 1] if moe else 1.0
                            P.op("dve", "scalar_tensor_tensor", X1[:, sgl, hs], py, scal, X1[:, sgl, hs], ALU.mult, ALU.add,
                                 reads=[rpy, r_RW, r_X1[sgl]], writes=[r_X1[sgl]])
                            ycount += 1
                    mcount += 1
                gi += 1
        for s in range(NB):
            P.dma("sp", ZP[row0 + s * 128:row0 + (s + 1) * 128, :], X1[:, s, :], reads=[r_X1[s]], writes=[r_out])
        P.barrier()
    print("stage F emitted: instructions", P.ninst, "arena peak", A.peak)


def emit_final(P, A, PS, dr, XN, out, T):
    P.barrier()
    A.release(0)
    epst = A.alloc([1], F32); r_eps = Res()
    P.op("dve", "memset", epst, EPS, writes=[r_eps])
    sel0, sel1, r_sel = emit_sel(P, A)
    fnrow = A.alloc([D], F32, parts=2); r_fn = Res()
    P.dma("sp", fnrow, dr["fnw"], writes=[r_fn])
    fnb = A.alloc([D], F32); r_fnb = Res()
    r_psb = Res()
    emit_bcast(P, PS, r_psb, sel0, r_sel, fnrow, r_fn, fnb, r_fnb, bank=7)
    xt = [A.alloc([D], F32) for _ in range(3)]; r_xt = [Res() for _ in range(3)]
    ot = [A.alloc([D], F32) for _ in range(3)]; r_ot = [Res() for _ in range(3)]
    junk = A.alloc([D], F32); r_junk = Res()
    ss = [A.alloc([1], F32) for _ in range(3)]; r_ss = [Res() for _ in range(3)]
    rs = [A.alloc([1], F32) for _ in range(3)]; r_rs = [Res() for _ in range(3)]
    r_out = Res()
    for s in range(T // 128):
        i = s % 3
        P.dma("sp", xt[i], XN[NCTX + s * 128:NCTX + (s + 1) * 128, :], writes=[r_xt[i]])
        P.op("dve", "scalar_tensor_tensor", junk, xt[i], 1.0, xt[i], ALU.mult, ALU.mult, accum_out=ss[i],
             reads=[r_xt[i]], writes=[r_ss[i], r_junk])
        emit_rstd(P, ss[i], r_ss[i], rs[i], r_rs[i], 1.0 / D, epst, r_eps)
        P.op("dve", "scalar_tensor_tensor", ot[i], xt[i], rs[i], fnb, ALU.mult, ALU.mult,
             reads=[r_xt[i], r_rs[i], r_fnb], writes=[r_ot[i]])
        P.dma("sp", out[s * 128:(s + 1) * 128, :], ot[i], reads=[r_ot[i]], writes=[r_out])
    P.wait_all("sp", [r_out])
    P.barrier()


def emit_cc(P, kind, src, dst, rows_per, reads, writes):
    nc = P.nc
    if "cc" not in P.sems:
        P.sems["cc"] = P.es.enter_context(nc.semaphore("ccsem"))
        P.cccnt = 0
    sem = P.sems["cc"]
    deps = P._deps("pool", reads, writes)
    P._emit_waits("pool", deps)
    nrows = src.shape[0]
    groups = [[0, 1], [2, 3], [4, 5], [6, 7]]
    op = ALU.bypass if kind == "AllGather" else ALU.add
    for i, r0 in enumerate(range(0, nrows, rows_per)):
        n = min(rows_per, nrows - r0)
        if kind == "AllGather":
            o = dst[i]
            o = o[0:2 * n, :]
        else:
            o = dst[r0:r0 + n, :]
        P.cccnt += 1
        P.ninst += 1
        P.q["pool"].append(lambda h, a=src[r0:r0 + n, :], o=o: h.collective_compute(
            kind, op, replica_groups=groups, ins=[a], outs=[o]).then_inc(sem, 1))
    dep = ("cc", P.cccnt)
    for r in reads:
        r.rd["cc"] = dep
    for w in writes:
        w.lw = dep
        w.rd = {}


def build_fused(T, stop_after=99):
    nc = bass.Bass("TRN2", target_bir_lowering=False)
    NT = T + NCTX
    dr = {}

    def din(name, shape):
        dr[name] = nc.dram_tensor(name, list(shape), F32, kind="ExternalInput").ap()

    din("xs", [T, D]); din("cx", [NCTX, D]); din("ccT", [128, 8, 2])
    din("cos", [T, 32]); din("sin", [T, 32])
    din("ident", [128, 128]); din("tri", [128, 256]); din("bigm", [128, 1024]); din("strict", [128, 2048])
    din("fnw", [2, D])
    FH = [1408, 3584]; NEH = [1, 4]
    for l in range(2):
        sfx = "_%d" % l
        din("wmodM" + sfx, [D, 2048]); din("bmodM" + sfx, [2, 2048]); din("nw1" + sfx, [2, D])
        din("wtm" + sfx, [D, 656]); din("wfm" + sfx, [D, 768]); din("convw" + sfx, [128, 6, 5])
        din("gp" + sfx, [128, 16]); din("qkw" + sfx, [128, 320]); din("dnw" + sfx, [128, 256])
        din("wmodF" + sfx, [D, 4096]); din("bmodF" + sfx, [2, 4096]); din("nw2" + sfx, [2, D])
        din("wout" + sfx, [D, D]); din("wg" + sfx, [NEH[l], D, FH[l]]); din("wu" + sfx, [NEH[l], D, FH[l]])
        din("wd" + sfx, [NEH[l], FH[l], D]); din("wr" + sfx, [D, 8]); din("rb" + sfx, [128, 8])
    out = nc.dram_tensor("out", [T, D], F32, kind="ExternalOutput").ap()
    mixA = nc.dram_tensor("mixA", [NT, 512], F32).ap()
    RPG = 1024
    ngc = (NT + RPG - 1) // RPG
    mixGc = nc.dram_tensor("mixGc", [ngc, 2 * RPG, 512], F32).ap()
    Zd = nc.dram_tensor("Zd", [NT, 256], F32).ap()
    REC = nc.dram_tensor("REC", [NT, 768], F32).ap()
    ZP = nc.dram_tensor("ZP", [NT, D], F32).ap()
    XN = [nc.dram_tensor("XN%d" % l, [NT, D], F32).ap() for l in range(2)]

    P = Prog(nc)
    A = Arena(P, 200 * 1024)
    PS = P.ps([128, 4096], F32)
    r_mixA = Res(); r_mixG = Res(); r_ZP = Res(); r_XN = [Res(), Res()]

    class MixG:
        def __getitem__(self, key):
            rs, cs = key
            r0, r1 = rs.start, rs.stop
            rank = r0 // NT
            r0 -= rank * NT; r1 -= rank * NT
            ci = r0 // RPG
            assert (r1 - 1) // RPG == ci
            nrows_c = min(RPG, NT - ci * RPG)
            base = rank * nrows_c + (r0 - ci * RPG)
            return mixGc[ci, base:base + (r1 - r0), cs]
    mixG = MixG()

    for l in range(2):
        sfx = "_%d" % l
        last = l == 1
        drM = dict(ccT=dr["ccT"], cos=dr["cos"], sin=dr["sin"], ident=dr["ident"], tri=dr["tri"], bigm=dr["bigm"],
                   strict=dr["strict"], wmod=dr["wmodM" + sfx], bmod=dr["bmodM" + sfx], nw=dr["nw1" + sfx],
                   wtm=dr["wtm" + sfx], wfm=dr["wfm" + sfx], convw=dr["convw" + sfx], gp=dr["gp" + sfx],
                   qkw=dr["qkw" + sfx], dnw=dr["dnw" + sfx])
        drF = dict(ccT=dr["ccT"], ident=dr["ident"], wmod=dr["wmodF" + sfx], bmod=dr["bmodF" + sfx], nw=dr["nw2" + sfx],
                   wout=dr["wout" + sfx], wg=dr["wg" + sfx], wu=dr["wu" + sfx], wd=dr["wd" + sfx], wr=dr["wr" + sfx],
                   rb=dr["rb" + sfx])
        if l == 0:
            def xrow(t0):
                return dr["cx"][t0:t0 + 128, :] if t0 < NCTX else dr["xs"][t0 - NCTX:t0 - NCTX + 128, :]
        else:
            def xrow(t0):
                return XN[0][t0:t0 + 128, :]
        emit_M(P, A, PS, drM, T, not last, xrow, mixA, Zd, REC)
        P.barrier()
        emit_cc(P, "AllGather", mixA, mixGc, RPG, [r_mixA], [r_mixG])
        P.barrier_cc()
        if stop_after == 2 * l + 1:
            r_o = Res()
            for t0 in range(0, T, 128):
                P.dma("sp", out[t0:t0 + 128, 0:512], mixG[NCTX + t0:NCTX + t0 + 128, :], writes=[r_o])
                P.dma("sp", out[t0:t0 + 128, 512:1024], mixG[NT + NCTX + t0:NT + NCTX + t0 + 128, :], writes=[r_o])
            P.wait_all("sp", [r_o])
            P.barrier()
            break
        blocks = [(NCTX + i * 2048, 2048, False) for i in range(T // 2048)]
        if not last:
            blocks = blocks + [(0, NCTX, True)]
        emit_F(P, A, PS, drF, blocks, 4 if last else 0, False, FH[l], NEH[l], xrow, mixG, NT, ZP)
        P.barrier()
        if last:
            emit_cc(P, "AllReduce", ZP[NCTX:NT, :], XN[l][NCTX:NT, :], 512, [r_ZP], [r_XN[l]])
        else:
            emit_cc(P, "AllReduce", ZP, XN[l], 512, [r_ZP], [r_XN[l]])
        P.barrier_cc()
        if stop_after == 2 * l + 2:
            r_o = Res()
            for t0 in range(0, T, 512):
                P.dma("sp", out[t0:t0 + 512, :], XN[l][NCTX + t0:NCTX + t0 + 512, :], writes=[r_o])
            P.wait_all("sp", [r_o])
            P.barrier()
            break
    if stop_after >= 99:
        emit_final(P, A, PS, dr, XN[1], out, T)
    print("fused: instructions", P.ninst, "arena peak", A.peak)
    P.finish()
    return nc


DN_DIM = 512
OFF_DN_Z = 1536
OFF_DN_A = 2048
OFF_DN_B = 2064
OFF_AT_Q = 2080
OFF_AT_K = 2592
OFF_AT_V = 2720


def consts():
    k = np.arange(128)
    U = (k[:, None] <= k[None, :]).astype(np.float32)
    L = (k[:, None] >= k[None, :]).astype(np.float32)
    tri = np.concatenate([U, L], axis=1)
    j = k[:, None]; i = k[None, :]
    big_f = np.where(i >= j, 0.0, -BIG).astype(np.float32)
    big_b = np.where(i <= j, 0.0, -BIG).astype(np.float32)
    st_f = (i > j).astype(np.float32)
    st_b = (i < j).astype(np.float32)
    bigm = np.concatenate([np.tile(big_f, (1, 4)), np.tile(big_b, (1, 4))], axis=1)
    same = ((i // 64) == (j // 64)).astype(np.float32)
    strict = np.concatenate([np.tile(st_f * same, (1, 4)), np.tile(st_f * (1 - same), (1, 4)),
                             np.tile(st_b * same, (1, 4)), np.tile(st_b * (1 - same), (1, 4))], axis=1)
    return dict(ident=np.eye(128, dtype=np.float32), tri=tri, bigm=bigm, strict=strict)


def rope_tables(T):
    rows = T // 64
    row_pos = np.repeat(np.arange(rows, dtype=np.float32), 64)[:T]
    col_pos = np.tile(np.arange(64, dtype=np.float32), rows)
    n_freq = 16
    freqs = (np.float32(10000.0) ** (-np.arange(n_freq, dtype=np.float32) / np.float32(n_freq))).astype(np.float32)
    ang = np.concatenate([row_pos[:, None] * freqs, col_pos[:, None] * freqs], axis=-1).astype(np.float32)
    return np.cos(ang).astype(np.float32), np.sin(ang).astype(np.float32)


def ccT_of(cb, c_ctx):
    cc = np.stack([cb, c_ctx], axis=0)
    return np.ascontiguousarray(cc.reshape(2, 8, 128).transpose(2, 1, 0)).astype(np.float32)


def prep_M(inp, layer, b, j, x_b, ctx_b, T, cst, cos, sin):
    w_in = inp["w_in"][layer]
    hq = slice(256 * j, 256 * j + 256)
    a_cols = [OFF_DN_A + d * 8 + 4 * j + h for d in range(2) for h in range(4)]
    b_cols = [OFF_DN_B + d * 8 + 4 * j + h for d in range(2) for h in range(4)]
    wtm = np.concatenate([
        w_in[:, OFF_AT_Q + 256 * j: OFF_AT_Q + 256 * j + 256],
        w_in[:, OFF_AT_K + 64 * j: OFF_AT_K + 64 * j + 64],
        w_in[:, OFF_AT_V + 64 * j: OFF_AT_V + 64 * j + 64],
        w_in[:, OFF_DN_Z + 256 * j: OFF_DN_Z + 256 * j + 256],
        w_in[:, a_cols], w_in[:, b_cols]], axis=1)
    wfm = np.concatenate([w_in[:, 0 + 256 * j: 256 * j + 256], w_in[:, 512 + 256 * j: 512 + 256 * j + 256],
                          w_in[:, 1024 + 256 * j: 1024 + 256 * j + 256]], axis=1)
    cw = inp["dn_conv_w"][layer]
    cw_my = np.concatenate([cw[:, 256 * j:256 * j + 256], cw[:, 512 + 256 * j:512 + 256 * j + 256],
                            cw[:, 1024 + 256 * j:1024 + 256 * j + 256]], axis=1)
    convw = np.ascontiguousarray(cw_my.reshape(5, 6, 128).transpose(2, 1, 0))
    dtb = inp["dn_dt_bias"][layer][:, 4 * j:4 * j + 4].reshape(8)
    alog = inp["dn_a_log"][layer][:, 4 * j:4 * j + 4].reshape(8)
    gp = np.tile(np.concatenate([dtb, alog])[None, :], (128, 1))
    qkw = np.tile(np.concatenate([np.tile(inp["at_qnorm_w"][layer], 4), inp["at_knorm_w"][layer]])[None, :], (128, 1))
    dnw = np.tile(np.tile(inp["dn_norm_w"][layer], 4)[None, :], (128, 1))
    m = dict(
        ccT=ccT_of(inp["c"][b], inp["c_ctx"]),
        wmod=inp["w_mod"][layer][:, 0:2048], bmod=np.tile(inp["b_mod"][layer][None, 0:2048], (2, 1)),
        nw=np.tile(inp["norm1_w"][layer][None, :], (2, 1)),
        wtm=wtm, wfm=wfm, convw=convw, gp=gp, qkw=qkw, dnw=dnw, cos=cos, sin=sin)
    m.update(cst)
    if x_b is not None:
        m["xs"] = x_b; m["cx"] = ctx_b
    return {k: np.ascontiguousarray(v, dtype=np.float32) for k, v in m.items()}


def prep_F(inp, layer, b, x_rows, mix_rows, cst, moe, final_norm):
    if moe:
        jm = layer // 2
        wg = inp["moe_w_gate"][jm]; wu = inp["moe_w_up"][jm]; wd = inp["moe_w_down"][jm]
        wr = inp["moe_router_w"][jm]; rb = np.tile(inp["moe_router_b"][jm][None, :], (128, 1))
    else:
        i = layer // 2
        wg = inp["ffn_w_gate"][i][None]; wu = inp["ffn_w_up"][i][None]; wd = inp["ffn_w_down"][i][None]
        wr = np.zeros((1024, 8), np.float32); rb = np.zeros((128, 8), np.float32)
    m = dict(x=x_rows, mixin=mix_rows, ccT=ccT_of(inp["c"][b], inp["c_ctx"]),
             wmod=inp["w_mod"][layer][:, 2048:6144], bmod=np.tile(inp["b_mod"][layer][None, 2048:6144], (2, 1)),
             nw=np.tile(inp["norm2_w"][layer][None, :], (2, 1)), fnw=np.tile(inp["final_norm_w"][None, :], (2, 1)),
             wout=inp["w_out"][layer], wg=wg, wu=wu, wd=wd, wr=wr, rb=rb, ident=cst["ident"])
    return {k: np.ascontiguousarray(v, dtype=np.float32) for k, v in m.items()}


def prep_fused(inp, b, j, T, cst, cos, sin):
    m = dict(xs=inp["x"][b], cx=inp["ctx"][b], ccT=ccT_of(inp["c"][b], inp["c_ctx"]), cos=cos, sin=sin,
             fnw=np.tile(inp["final_norm_w"][None, :], (2, 1)))
    m.update(cst)
    for l in range(2):
        sfx = "_%d" % l
        pm = prep_M(inp, l, b, j, None, None, T, {}, cos, sin)
        m["wmodM" + sfx] = pm["wmod"]; m["bmodM" + sfx] = pm["bmod"]; m["nw1" + sfx] = pm["nw"]
        for k in ("wtm", "wfm", "convw", "gp", "qkw", "dnw"):
            m[k + sfx] = pm[k]
        m["wmodF" + sfx] = inp["w_mod"][l][:, 2048:6144]
        m["bmodF" + sfx] = np.tile(inp["b_mod"][l][None, 2048:6144], (2, 1))
        m["nw2" + sfx] = np.tile(inp["norm2_w"][l][None, :], (2, 1))
        wo = inp["w_out"][l]
        m["wout" + sfx] = np.concatenate([wo[0:256], wo[512:768], wo[256:512], wo[768:1024]], axis=0)
        if l % 2 == 0:
            i = l // 2
            sl = slice(1408 * j, 1408 * (j + 1))
            m["wg" + sfx] = inp["ffn_w_gate"][i][:, sl][None]
            m["wu" + sfx] = inp["ffn_w_up"][i][:, sl][None]
            m["wd" + sfx] = inp["ffn_w_down"][i][sl, :][None]
            m["wr" + sfx] = np.zeros((1024, 8), np.float32)
            m["rb" + sfx] = np.zeros((128, 8), np.float32)
        else:
            jm = l // 2
            own = list(range(4 * j, 4 * j + 4))
            perm = own + [e for e in range(8) if e not in own]
            m["wg" + sfx] = inp["moe_w_gate"][jm][4 * j:4 * j + 4]
            m["wu" + sfx] = inp["moe_w_up"][jm][4 * j:4 * j + 4]
            m["wd" + sfx] = inp["moe_w_down"][jm][4 * j:4 * j + 4]
            m["wr" + sfx] = inp["moe_router_w"][jm][:, perm]
            m["rb" + sfx] = np.tile(inp["moe_router_b"][jm][perm][None, :], (128, 1))
    return {k: np.ascontiguousarray(v, dtype=np.float32) for k, v in m.items()}


def kernel(**inputs):
    from concourse.bass_utils import run_bass_kernel_spmd
    inp = {k: np.asarray(v, dtype=np.float32) for k, v in inputs.items()}
    Bn, T = inp["x"].shape[0], inp["x"].shape[1]
    cst = consts()
    cos, sin = rope_tables(T)
    nc = build_fused(T)
    maps = [prep_fused(inp, core // 2, core % 2, T, cst, cos, sin) for core in range(2 * Bn)]
    res = run_bass_kernel_spmd(nc, maps, core_ids=list(range(2 * Bn))).results
    out = np.stack([res[2 * b]["out"] for b in range(Bn)], axis=0)
    return out.astype(np.float32)
```

```python
import contextlib
import numpy as np
import concourse.bass as bass
import concourse.mybir as mybir

F32 = mybir.dt.float32
BF16 = mybir.dt.bfloat16
ALU = mybir.AluOpType
AF = mybir.ActivationFunctionType
AX = mybir.AxisListType


class Res:
    __slots__ = ("name", "lw", "rd")

    def __init__(self, name=""):
        self.name = name
        self.lw = None
        self.rd = {}


class Prog:
    ENG = ("pe", "act", "dve", "pool", "sp")
    NDMA = 10

    def __init__(self, nc):
        self.nc = nc
        self.es = contextlib.ExitStack()
        self.q = {e: [] for e in self.ENG}
        self.cnt = {e: 0 for e in self.ENG}
        self.sems = {}
        for e in self.ENG:
            self.sems[e] = self.es.enter_context(nc.semaphore("c_" + e))
        self.dcnt = {}
        self.dnext = {}
        for qn in ("sp", "pool", "act"):
            for i in range(self.NDMA):
                k = "d_%s_%d" % (qn, i)
                self.sems[k] = self.es.enter_context(nc.semaphore(k))
                self.dcnt[k] = 0
            self.dnext[qn] = 0
        self.waited = {}
        self.ntile = 0
        self.ninst = 0

    def sb(self, shape, dt, name=None):
        self.ntile += 1
        name = name or ("t%d" % self.ntile)
        return self.es.enter_context(self.nc.sbuf_tensor(name, list(shape), dt))

    def ps(self, shape, dt, name=None):
        self.ntile += 1
        name = name or ("p%d" % self.ntile)
        return self.es.enter_context(self.nc.psum_tensor(name, list(shape), dt))

    def _need(self, eng, dep, out):
        if dep is None:
            return
        key, val = dep
        if key == "pe" and eng == "pe":
            return
        if self.waited.get((eng, key), 0) >= val:
            return
        self.waited[(eng, key)] = val
        out.append((key, val))

    def _deps(self, eng, reads, writes):
        out = []
        for r in reads:
            self._need(eng, r.lw, out)
        for w in writes:
            self._need(eng, w.lw, out)
            for k, d in w.rd.items():
                self._need(eng, d, out)
        return out

    def _emit_waits(self, eng, deps):
        sems = self.sems
        for key, val in deps:
            self.q[eng].append(lambda h, key=key, val=val: h.wait_ge(sems[key], val))
            self.ninst += 1

    def op(self, eng, meth, *args, reads=(), writes=(), inc=True, **kw):
        deps = self._deps(eng, reads, writes)
        self._emit_waits(eng, deps)
        self.ninst += 1
        if inc:
            self.cnt[eng] += 1
            val = self.cnt[eng]
            sem = self.sems[eng]
            self.q[eng].append(lambda h: getattr(h, meth)(*args, **kw).then_inc(sem, 1))
        else:
            val = self.cnt[eng] + 1
            self.q[eng].append(lambda h: getattr(h, meth)(*args, **kw))
        dep = (eng, val)
        for r in reads:
            r.rd[eng] = dep
        for w in writes:
            w.lw = dep
            w.rd = {}
        return dep

    def dma(self, qn, out, in_, reads=(), writes=(), **kw):
        i = self.dnext[qn]
        self.dnext[qn] = (i + 1) % self.NDMA
        k = "d_%s_%d" % (qn, i)
        deps = self._deps(qn, reads, writes)
        if self.dcnt[k] > 0:
            self._need(qn, (k, self.dcnt[k]), deps)
        self._emit_waits(qn, deps)
        self.dcnt[k] += 16
        val = self.dcnt[k]
        sem = self.sems[k]
        self.ninst += 1
        self.q[qn].append(lambda h: h.dma_start(out=out, in_=in_, **kw).then_inc(sem, 16))
        dep = (k, val)
        for r in reads:
            r.rd[k] = dep
        for w in writes:
            w.lw = dep
            w.rd = {}
        return dep

    def wait_all(self, eng, resources):
        out = []
        for r in resources:
            self._need(eng, r.lw, out)
        self._emit_waits(eng, out)

    def barrier(self):
        for e in self.ENG:
            out = []
            for e2 in self.ENG:
                if e2 != e and self.cnt[e2] > 0:
                    self._need(e, (e2, self.cnt[e2]), out)
            for k, v in self.dcnt.items():
                if v > 0:
                    self._need(e, (k, v), out)
            self._emit_waits(e, out)

    def barrier_cc(self):
        if "cc" in self.sems and getattr(self, "cccnt", 0) > 0:
            for e in self.ENG:
                out = []
                self._need(e, ("cc", self.cccnt), out)
                self._emit_waits(e, out)
        self.barrier()

    def finish(self):
        nc = self.nc
        q = self.q
        with nc.Block() as block:
            @block.tensor
            def _(h):
                for f in q["pe"]:
                    f(h)

            @block.scalar
            def _(h):
                for f in q["act"]:
                    f(h)

            @block.vector
            def _(h):
                for f in q["dve"]:
                    f(h)

            @block.gpsimd
            def _(h):
                for f in q["pool"]:
                    f(h)

            @block.sync
            def _(h):
                for f in q["sp"]:
                    f(h)
        self.es.close()


class Arena:
    def __init__(self, P, nbytes, name="arena"):
        self.t = P.sb([128, nbytes // 4], F32, name=name)
        self.nbytes = nbytes
        self.off = 0
        self.peak = 0

    def alloc(self, free_shape, dt, parts=128):
        esz = 4 if dt == F32 else 2
        n = int(np.prod(free_shape))
        nb = (n * esz + 31) // 32 * 32
        assert self.off + nb <= self.nbytes, "arena overflow %d + %d > %d" % (self.off, nb, self.nbytes)
        w0 = self.off // 4
        ap = self.t[0:parts, w0:w0 + nb // 4]
        if dt != F32:
            ap = ap.bitcast(dt)
        ap = ap[:, 0:n]
        self.off += nb
        self.peak = max(self.peak, self.off)
        if len(free_shape) == 2:
            ap = ap.rearrange("p (a b) -> p a b", a=free_shape[0])
        elif len(free_shape) == 3:
            ap = ap.rearrange("p (a b c) -> p a b c", a=free_shape[0], b=free_shape[1])
        return ap

    def mark(self):
        return self.off

    def release(self, m):
        self.off = m


D = 1024
NCTX = 256
EPS = 1e-6
BIG = 30000.0


def emit_mod(P, A, PS, dram, nchunk, consts):
    ccT = A.alloc([8, 2], F32); r_cc = Res()
    P.dma("sp", ccT, dram["ccT"], writes=[r_cc])
    P.op("act", "activation", ccT, ccT, AF.Silu, reads=[r_cc], writes=[r_cc])
    modrow = A.alloc([nchunk * 1024], F32, parts=2); r_mod = Res()
    m0 = A.mark()
    wbuf = [A.alloc([8, 512], F32) for _ in range(2)]
    bmb = [A.alloc([512], F32, parts=2) for _ in range(2)]
    r_w = [Res(), Res()]
    r_bm = [Res(), Res()]
    r_ps = [Res(), Res()]
    for g in range(nchunk * 2):
        wb = wbuf[g % 2]
        c0 = g * 512
        P.dma("sp", wb, dram["wmod"][:, c0:c0 + 512].rearrange("(c k) n -> k c n", k=128), writes=[r_w[g % 2]])
        P.dma("sp", bmb[g % 2], dram["bmod"][:, c0:c0 + 512], writes=[r_bm[g % 2]])
        pb = PS[0:2, (g % 2) * 512:(g % 2 + 1) * 512]
        for kc in range(8):
            P.op("pe", "matmul", pb, ccT[:, kc, :], wb[:, kc, :],
                 start=(kc == 0), stop=(kc == 7), reads=[r_cc, r_w[g % 2]], writes=[r_ps[g % 2]], inc=(kc == 7))
        P.op("dve", "tensor_tensor", modrow[:, c0:c0 + 512], pb, bmb[g % 2], ALU.add,
             reads=[r_ps[g % 2], r_bm[g % 2]], writes=[r_mod])
    P.barrier()
    A.release(m0)
    return modrow, r_mod


def emit_bcast(P, PS, r_psb, sel, r_sel, row_ap, r_row, out_ap, r_out, n=1024, bank=0):
    for g in range(n // 512):
        pb = PS[:, bank * 512:(bank + 1) * 512]
        P.op("pe", "matmul", pb, sel, row_ap[:, g * 512:(g + 1) * 512], start=True, stop=True,
             reads=[r_sel, r_row], writes=[r_psb])
        P.op("dve", "tensor_copy", out_ap[:, g * 512:(g + 1) * 512], pb, reads=[r_psb], writes=[r_out])


def emit_sel(P, A):
    sel0 = A.alloc([128], F32, parts=2)
    sel1 = A.alloc([128], F32, parts=2)
    r = Res()
    P.op("dve", "memset", sel0, 0.0, writes=[r])
    P.op("dve", "memset", sel0[0:1, :], 1.0, writes=[r])
    P.op("dve", "memset", sel1, 1.0, writes=[r])
    P.op("dve", "memset", sel1[0:1, :], 0.0, writes=[r])
    return sel0, sel1, r


def emit_rstd(P, ss, r_ss, out, r_out, scale, eps_ap, r_eps, premul=None):
    P.op("act", "activation", out, ss, AF.Ln, bias=eps_ap, scale=scale, reads=[r_ss, r_eps], writes=[r_out])
    P.op("act", "activation", out, out, AF.Exp, scale=-0.5, reads=[r_out], writes=[r_out])


def emit_M(P, A, PS, dr, T, want_ctx_out, xrow, mix, Zd, REC, do_attn=True, do_dn=True, dn_stage=9):
    debug = False
    dbg = None
    NT = T + NCTX
    nkt = NT // 128
    P.barrier()
    A.release(0)

    def bank(i, n=512):
        return PS[:, i * 512:i * 512 + n]

    def bankb(i, n=1024):
        return PS[:, i * 512:(i + 1) * 512].bitcast(BF16)[:, 0:n]

    ident = A.alloc([128], F32); r_ident = Res()
    identb = A.alloc([128], BF16); r_identb = Res()
    P.dma("sp", ident, dr["ident"], writes=[r_ident])
    P.dma("pool", identb, dr["ident"], writes=[r_identb])
    epst = A.alloc([1], F32); r_eps = Res()
    P.op("dve", "memset", epst, EPS, writes=[r_eps])

    GB = A.alloc([nkt, 16], F32); r_GB = Res()
    m_att = A.mark()
    QT = A.alloc([2, T], BF16); r_QT = Res()
    KT = A.alloc([NT], BF16); r_KT = Res()
    VA = A.alloc([nkt, 96], BF16); r_VA = Res()
    QTc = A.alloc([2, NCTX], BF16); r_QTc = Res()
    P.op("pool", "memset", VA, 0.0, writes=[r_VA])
    P.op("pool", "memset", VA[:, :, 64:65], 1.0, writes=[r_VA])
    m_p1 = A.mark()

    sel0, sel1, r_sel = emit_sel(P, A)
    modrow, r_mod = emit_mod(P, A, PS, dr, 2, None)
    nwrow = A.alloc([D], F32, parts=2); r_nw = Res()
    P.dma("sp", nwrow, dr["nw"], writes=[r_nw])
    weff = A.alloc([D], F32); r_weff = Res()
    shb = A.alloc([D], F32); r_shb = Res()
    r_psb = Res()

    def make_mod_tiles(sel):
        emit_bcast(P, PS, r_psb, sel, r_sel, modrow[:, 1024:2048], r_mod, weff, r_weff, bank=2)
        P.op("dve", "tensor_scalar", weff, weff, 1.0, None, ALU.add, reads=[r_weff], writes=[r_weff])
        emit_bcast(P, PS, r_psb, sel, r_sel, nwrow, r_nw, shb, r_shb, bank=2)
        P.op("dve", "tensor_tensor", weff, weff, shb, ALU.mult, reads=[r_weff, r_shb], writes=[r_weff])
        emit_bcast(P, PS, r_psb, sel, r_sel, modrow[:, 0:1024], r_mod, shb, r_shb, bank=2)

    Wtm = A.alloc([8, 656], BF16); r_Wtm = Res()
    Wfm = A.alloc([8, 768], BF16); r_Wfm = Res()
    P.dma("pool", Wtm, dr["wtm"].rearrange("(c k) n -> k c n", k=128), writes=[r_Wtm])
    P.dma("pool", Wfm, dr["wfm"].rearrange("(c k) n -> k c n", k=128), writes=[r_Wfm])
    convw = A.alloc([6, 5], F32); r_convw = Res()
    P.dma("sp", convw, dr["convw"], writes=[r_convw])
    gp = A.alloc([16], F32); r_gp = Res()
    P.dma("sp", gp, dr["gp"], writes=[r_gp])
    P.op("act", "activation", gp[:, 8:16], gp[:, 8:16], AF.Exp, reads=[r_gp], writes=[r_gp])
    P.op("dve", "tensor_scalar", gp[:, 8:16], gp[:, 8:16], -1.0, None, ALU.mult, reads=[r_gp], writes=[r_gp])
    qkw = A.alloc([5, 64], F32); r_qkw = Res()
    P.dma("sp", qkw, dr["qkw"].rearrange("p (a b) -> p a b", a=5), writes=[r_qkw])

    xt = [A.alloc([D], F32) for _ in range(2)]; r_xt = [Res(), Res()]
    htmp = A.alloc([D], F32); r_htmp = Res()
    hb = A.alloc([D], BF16); r_hb = Res()
    hT = A.alloc([8, 512], BF16); r_hT = Res()
    XR = [A.alloc([6, 516], F32) for _ in range(2)]; r_XR = [Res(), Res()]
    cacc = A.alloc([6, 512], F32); r_cacc = Res()
    sact = A.alloc([6, 512], BF16); r_sact = Res()
    tms = A.alloc([656], F32); r_tms = Res()
    ss1 = A.alloc([1], F32); r_ss1 = Res()
    rs1 = A.alloc([1], F32); r_rs1 = Res()
    junk = A.alloc([D], F32)
    r_junk = Res()
    sq5 = A.alloc([5, 64], F32); r_sq5 = Res()
    ss5 = A.alloc([5], F32); r_ss5 = Res()
    rs5 = A.alloc([5], F32); r_rs5 = Res()
    qn = A.alloc([5, 64], F32); r_qn = Res()
    tt = [A.alloc([5, 32], F32) for _ in range(4)]; r_tt = Res()
    qr = A.alloc([6, 64], BF16); r_qr = Res()
    cst = A.alloc([2, 32], F32); r_cst = Res()
    gt = A.alloc([16], F32); r_gt = Res()
    rec = A.alloc([768], F32); r_rec = Res()
    sq8 = A.alloc([512], F32); r_sq8 = Res()
    ss8 = A.alloc([8], F32); r_ss8 = Res()
    rs8 = A.alloc([8], F32); r_rs8 = Res()

    r_pT = [Res(), Res()]
    r_ptm = Res()
    r_pfm = [Res(), Res()]
    r_pat = Res()
    r_pdn = Res()
    r_Zd = Res(); r_REC = Res(); r_dbg = Res()

    macros = [(0, NCTX, True)] + [(NCTX + i * 512, 512, False) for i in range(T // 512)]
    nsub_done = 0

    def conv_macro(mi):
        s0, n, is_ctx = macros[mi]
        X = XR[mi % 2]; rX = r_XR[mi % 2]
        for ch in range(6):
            eng = "dve"
            P.op(eng, "tensor_scalar", cacc[:, ch, 0:n], X[:, ch, 0:n], convw[:, ch, 0:1], None, ALU.mult,
                 reads=[rX, r_convw], writes=[r_cacc])
            for k in range(1, 5):
                P.op(eng, "scalar_tensor_tensor", cacc[:, ch, 0:n], X[:, ch, k:k + n], convw[:, ch, k:k + 1],
                     cacc[:, ch, 0:n], ALU.mult, ALU.add, reads=[rX, r_convw, r_cacc], writes=[r_cacc])
        P.op("act", "activation", sact[:, :, 0:n], cacc[:, :, 0:n], AF.Silu, reads=[r_cacc], writes=[r_sact])
        for sub in range(n // 128):
            t0 = s0 + sub * 128
            pb = bankb(7, 768)
            for ch in range(6):
                P.op("pe", "transpose", pb[:, ch * 128:(ch + 1) * 128], sact[:, ch, sub * 128:(sub + 1) * 128], identb,
                     reads=[r_sact, r_identb], writes=[r_pdn], inc=(ch == 5))
            P.op("act", "activation", rec, pb, AF.Copy, reads=[r_pdn], writes=[r_rec])
            P.op("dve", "tensor_tensor", sq8, rec[:, 0:512], rec[:, 0:512], ALU.mult, reads=[r_rec], writes=[r_sq8])
            P.op("dve", "tensor_reduce", ss8, sq8.rearrange("p (a b) -> p a b", a=8), AX.X, ALU.add,
                 reads=[r_sq8], writes=[r_ss8])
            emit_rstd(P, ss8, r_ss8, rs8, r_rs8, 1.0, epst, r_eps)
            P.op("dve", "tensor_scalar", rs8[:, 0:4], rs8[:, 0:4], 0.125, None, ALU.mult, reads=[r_rs8], writes=[r_rs8])
            P.op("dve", "tensor_tensor", rec[:, 0:512].rearrange("p (a b) -> p a b", a=8),
                 rec[:, 0:512].rearrange("p (a b) -> p a b", a=8),
                 rs8.unsqueeze(2).to_broadcast([128, 8, 64]), ALU.mult, reads=[r_rec, r_rs8], writes=[r_rec])
            P.dma("sp", REC[t0:t0 + 128, :], rec, reads=[r_rec], writes=[r_REC])

    for mi, (s0, n, is_ctx) in enumerate(macros):
        if mi == 0:
            make_mod_tiles(sel1)
            if debug:
                P.dma("sp", dbg["d_mod"], modrow, reads=[r_mod], writes=[r_dbg])
                P.dma("sp", dbg["d_weff"], weff, reads=[r_weff], writes=[r_dbg])
                P.dma("sp", dbg["d_shb"], shb, reads=[r_shb], writes=[r_dbg])
        if mi == 1:
            make_mod_tiles(sel0)
        nsub = n // 128
        X = XR[mi % 2]; rX = r_XR[mi % 2]
        for sub in range(nsub):
            t0 = s0 + sub * 128
            kt = t0 // 128
            xb_ = xt[nsub_done % 2]; rx = r_xt[nsub_done % 2]
            src = xrow(t0)
            P.dma("sp", xb_, src, writes=[rx])
            P.op("dve", "scalar_tensor_tensor", junk, xb_, 1.0, xb_, ALU.mult, ALU.mult, accum_out=ss1,
                 reads=[rx], writes=[r_ss1, r_junk])
            emit_rstd(P, ss1, r_ss1, rs1, r_rs1, 1.0 / D, epst, r_eps)
            P.op("dve", "scalar_tensor_tensor", htmp, xb_, rs1, weff, ALU.mult, ALU.mult,
                 reads=[rx, r_rs1, r_weff], writes=[r_htmp])
            P.op("pool", "tensor_tensor", hb, htmp, shb, ALU.add, reads=[r_htmp, r_shb], writes=[r_hb])
            if debug:
                P.op("pool", "tensor_copy", junk, hb, reads=[r_hb], writes=[r_junk])
                P.dma("sp", dbg["d_h"][t0:t0 + 128, :], junk, reads=[r_junk], writes=[r_dbg])
            pb = bankb(nsub_done % 2, 1024); rp = r_pT[nsub_done % 2]
            for kc in range(8):
                P.op("pe", "transpose", pb[:, kc * 128:(kc + 1) * 128], hb[:, kc * 128:(kc + 1) * 128], identb,
                     reads=[r_hb, r_identb], writes=[rp], inc=(kc == 7))
            P.op("act", "activation", hT[:, :, sub * 128:(sub + 1) * 128], pb.rearrange("p (a b) -> p a b", a=8), AF.Copy,
                 reads=[rp], writes=[r_hT])
            for g, (c0, c1) in enumerate(((0, 512), (512, 656))):
                pbm = PS[:, (2 + g) * 512:(2 + g) * 512 + (c1 - c0)]
                for kc in range(8):
                    P.op("pe", "matmul", pbm, hT[:, kc, sub * 128:(sub + 1) * 128], Wtm[:, kc, c0:c1],
                         start=(kc == 0), stop=(kc == 7), reads=[r_hT, r_Wtm], writes=[r_ptm], inc=(kc == 7 and g == 1))
            P.op("act", "activation", tms[:, 0:512], bank(2), AF.Copy, reads=[r_ptm], writes=[r_tms])
            P.op("act", "activation", tms[:, 512:656], bank(3, 144), AF.Copy, reads=[r_ptm], writes=[r_tms])
            if debug:
                P.dma("sp", dbg["d_tm"][t0:t0 + 128, :], tms, reads=[r_tms], writes=[r_dbg])
            P.dma("sp", Zd[t0:t0 + 128, :], tms[:, 384:640], reads=[r_tms], writes=[r_Zd])
            P.op("dve", "tensor_tensor", gt[:, 0:8], tms[:, 640:648], gp[:, 0:8], ALU.add, reads=[r_tms, r_gp], writes=[r_gt])
            P.op("act", "activation", gt[:, 0:8], gt[:, 0:8], AF.Exp, reads=[r_gt], writes=[r_gt])
            P.op("act", "activation", gt[:, 0:8], gt[:, 0:8], AF.Ln, bias=1.0, reads=[r_gt], writes=[r_gt])
            P.op("dve", "tensor_tensor", GB[:, kt, 0:8], gt[:, 0:8], gp[:, 8:16], ALU.mult, reads=[r_gt, r_gp], writes=[r_GB])
            P.op("act", "activation", gt[:, 8:16], tms[:, 648:656], AF.Exp, scale=-1.0, reads=[r_tms], writes=[r_gt])
            P.op("dve", "tensor_scalar", gt[:, 8:16], gt[:, 8:16], 1.0, None, ALU.add, reads=[r_gt], writes=[r_gt])
            P.op("dve", "reciprocal", GB[:, kt, 8:16], gt[:, 8:16], reads=[r_gt], writes=[r_GB])
            qk3 = tms[:, 0:320].rearrange("p (a b) -> p a b", a=5)
            P.op("dve", "tensor_tensor", sq5, qk3, qk3, ALU.mult, reads=[r_tms], writes=[r_sq5])
            P.op("dve", "tensor_reduce", ss5, sq5, AX.X, ALU.add, reads=[r_sq5], writes=[r_ss5])
            emit_rstd(P, ss5, r_ss5, rs5, r_rs5, 1.0 / 64, epst, r_eps)
            P.op("dve", "tensor_scalar", rs5[:, 0:4], rs5[:, 0:4], 0.125, None, ALU.mult, reads=[r_rs5], writes=[r_rs5])
            P.op("dve", "tensor_tensor", qn, qk3, rs5.unsqueeze(2).to_broadcast([128, 5, 64]), ALU.mult,
                 reads=[r_tms, r_rs5], writes=[r_qn])
            if is_ctx:
                P.op("dve", "tensor_tensor", qr[:, 0:5, :], qn, qkw, ALU.mult, reads=[r_qn, r_qkw], writes=[r_qr])
            else:
                P.op("dve", "tensor_tensor", qn, qn, qkw, ALU.mult, reads=[r_qn, r_qkw], writes=[r_qn])
                tl = t0 - NCTX
                P.dma("sp", cst[:, 0, :], dr["cos"][tl:tl + 128, :], writes=[r_cst])
                P.dma("sp", cst[:, 1, :], dr["sin"][tl:tl + 128, :], writes=[r_cst])
                cosb = cst[:, 0:1, :].to_broadcast([128, 5, 32])
                sinb = cst[:, 1:2, :].to_broadcast([128, 5, 32])
                x1 = qn[:, :, 0:32]; x2 = qn[:, :, 32:64]
                P.op("dve", "tensor_tensor", tt[0], x1, cosb, ALU.mult, reads=[r_qn, r_cst], writes=[r_tt])
                P.op("pool", "tensor_tensor", tt[1], x2, sinb, ALU.mult, reads=[r_qn, r_cst], writes=[r_tt])
                P.op("dve", "tensor_tensor", tt[2], x2, cosb, ALU.mult, reads=[r_qn, r_cst], writes=[r_tt])
                P.op("pool", "tensor_tensor", tt[3], x1, sinb, ALU.mult, reads=[r_qn, r_cst], writes=[r_tt])
                P.op("dve", "tensor_tensor", qr[:, 0:5, 0:32], tt[0], tt[1], ALU.subtract, reads=[r_tt], writes=[r_qr])
                P.op("dve", "tensor_tensor", qr[:, 0:5, 32:64], tt[2], tt[3], ALU.add, reads=[r_tt], writes=[r_qr])
            P.op("pool", "tensor_copy", qr[:, 5, :], qr[:, 4, :], reads=[r_qr], writes=[r_qr])
            P.op("pool", "tensor_copy", VA[:, kt, 0:64], tms[:, 320:384], reads=[r_tms], writes=[r_VA])
            pa = bankb(6, 384)
            qr2 = qr.rearrange("p a b -> p (a b)")
            for i in range(3):
                P.op("pe", "transpose", pa[:, i * 128:(i + 1) * 128], qr2[:, i * 128:(i + 1) * 128], identb,
                     reads=[r_qr, r_identb], writes=[r_pat], inc=(i == 2))
            if is_ctx:
                P.op("dve", "tensor_copy", QTc[:, :, t0:t0 + 128], pa[:, 0:256].rearrange("p (a b) -> p a b", a=2),
                     reads=[r_pat], writes=[r_QTc])
            else:
                tl = t0 - NCTX
                P.op("dve", "tensor_copy", QT[:, :, tl:tl + 128], pa[:, 0:256].rearrange("p (a b) -> p a b", a=2),
                     reads=[r_pat], writes=[r_QT])
            P.op("dve", "tensor_copy", KT[:, t0:t0 + 128], pa[:, 256:384], reads=[r_pat], writes=[r_KT])
            nsub_done += 1
        for ch in range(6):
            pbf = PS[:, (4 + ch % 2) * 512:(4 + ch % 2) * 512 + n]; rp = r_pfm[ch % 2]
            for kc in range(8):
                P.op("pe", "matmul", pbf, Wfm[:, kc, ch * 128:(ch + 1) * 128], hT[:, kc, 0:n],
                     start=(kc == 0), stop=(kc == 7), reads=[r_hT, r_Wfm], writes=[rp], inc=(kc == 7))
            P.op("act", "activation", X[:, ch, 2:2 + n], pbf, AF.Copy, reads=[rp], writes=[rX])
        prev_same = mi >= 2
        if prev_same:
            Xp = XR[(mi - 1) % 2]; rXp = r_XR[(mi - 1) % 2]
            P.op("pool", "tensor_copy", Xp[:, :, 514:516], X[:, :, 2:4], reads=[rX], writes=[rXp])
            P.op("pool", "tensor_copy", X[:, :, 0:2], Xp[:, :, 512:514], reads=[rXp], writes=[rX])
        else:
            P.op("pool", "memset", X[:, :, 0:2], 0.0, writes=[rX])
            if mi == 1:
                Xp = XR[0]; rXp = r_XR[0]
                P.op("pool", "memset", Xp[:, :, 2 + NCTX:4 + NCTX], 0.0, writes=[rXp])
        if mi >= 1:
            conv_macro(mi - 1)
    lastm = len(macros) - 1
    nl = macros[lastm][1]
    P.op("pool", "memset", XR[lastm % 2][:, :, 2 + nl:4 + nl], 0.0, writes=[r_XR[lastm % 2]])
    conv_macro(lastm)

    if debug:
        P.barrier()
        r_o = Res()
        st = A.alloc([2 * T], F32)
        P.op("dve", "tensor_copy", st, QT.rearrange("p a b -> p (a b)"), writes=[r_o])
        P.dma("sp", dbg["d_qt"].rearrange("p a b -> p (a b)"), st, reads=[r_o], writes=[r_dbg])
        st2 = A.alloc([NT], F32)
        P.op("dve", "tensor_copy", st2, KT, writes=[r_o])
        P.dma("sp", dbg["d_kt"], st2, reads=[r_o], writes=[r_dbg])
        st3 = A.alloc([nkt, 65], F32)
        P.op("dve", "tensor_copy", st3, VA, writes=[r_o])
        P.dma("sp", dbg["d_v"], st3, reads=[r_o], writes=[r_dbg])
        P.dma("sp", dbg["d_gb"], GB, reads=[r_o], writes=[r_dbg])
        st4 = A.alloc([2, NCTX], F32)
        P.op("dve", "tensor_copy", st4, QTc, writes=[r_o])
        P.dma("sp", dbg["d_qtc"], st4, reads=[r_o], writes=[r_dbg])

    P.barrier()
    A.release(m_p1)
    r_mix = Res()

    m_p2 = A.mark()
    PT = [A.alloc([1024], BF16) for _ in range(3)]; r_PT = [Res() for _ in range(3)]
    osb = A.alloc([512], F32, parts=65); r_osb = Res()
    ost = A.alloc([4, 64], F32); r_ost = Res()
    rcp = A.alloc([4], F32); r_rcp = Res()
    r_pS = [Res(), Res()]; r_pO = [Res() for _ in range(4)]; r_pTr = Res()
    nit = 0
    nq = 0

    def attn_block(q2, nqtok, ktiles, row0, col0):
        nonlocal nit, nq
        pset = 4 + 2 * (nq % 2)
        po = [PS[0:96, (pset + hf) * 512:(pset + hf) * 512 + nqtok] for hf in range(2)]
        rpo = [r_pO[2 * (nq % 2) + hf] for hf in range(2)]
        nk = len(ktiles)

        def emit_pv(ki, it):
            pt = PT[it % 3]; rpt = r_PT[it % 3]
            kt = ktiles[ki]
            for hf in range(2):
                P.op("pe", "matmul", po[hf], VA[:, kt, :], pt[:, hf * 512:hf * 512 + nqtok],
                     start=(ki == 0), stop=(ki == nk - 1), reads=[rpt, r_VA], writes=[rpo[hf]], inc=(ki == nk - 1))

        for ki in range(nk):
            kt = ktiles[ki]
            sb_ = nit % 2
            pS = PS[:, sb_ * 1024:(sb_ + 1) * 1024]; rS = r_pS[sb_]
            for hf in range(2):
                P.op("pe", "matmul", pS[:, hf * 512:hf * 512 + nqtok], KT[hf * 64:(hf + 1) * 64, kt * 128:(kt + 1) * 128],
                     q2[hf * 64:(hf + 1) * 64, :], start=True, stop=True, reads=[r_KT, r_QT, r_QTc], writes=[rS], inc=(hf == 1))
            if ki >= 1:
                emit_pv(ki - 1, nit - 1)
            pt = PT[nit % 3]; rpt = r_PT[nit % 3]
            if nqtok == 512:
                P.op("act", "activation", pt, pS, AF.Exp, reads=[rS], writes=[rpt])
            else:
                P.op("act", "activation", pt.rearrange("p (u n) -> p u n", u=2)[:, :, 0:nqtok],
                     pS.rearrange("p (u n) -> p u n", u=2)[:, :, 0:nqtok], AF.Exp, reads=[rS], writes=[rpt])
            nit += 1
        emit_pv(nk - 1, nit - 1)
        nsub = nqtok // 128
        for hf in range(2):
            P.op("dve", "tensor_copy", osb[:, 0:nqtok], po[hf][0:65, :], reads=[rpo[hf]], writes=[r_osb])
            ptr = PS[:, (pset + hf) * 512:(pset + hf) * 512 + 4 * 65]
            for sub in range(nsub):
                P.op("pe", "transpose", ptr[:, sub * 65:(sub + 1) * 65], osb[:, sub * 128:(sub + 1) * 128], ident[0:65, 0:65],
                     reads=[r_osb, r_ident], writes=[rpo[hf]], inc=(sub == nsub - 1))
            ptr3 = ptr.rearrange("p (s c) -> p s c", s=4)
            P.op("dve", "reciprocal", rcp[:, 0:nsub], ptr3[:, 0:nsub, 64], reads=[rpo[hf]], writes=[r_rcp])
            P.op("dve", "tensor_tensor", ost[:, 0:nsub, :], ptr3[:, 0:nsub, 0:64],
                 rcp[:, 0:nsub].unsqueeze(2).to_broadcast([128, nsub, 64]), ALU.mult, reads=[rpo[hf], r_rcp], writes=[r_ost])
            P.dma("sp", mix[row0:row0 + nqtok, col0 + hf * 64:col0 + hf * 64 + 64].rearrange("(s p) c -> p s c", p=128),
                  ost[:, 0:nsub, :], reads=[r_ost], writes=[r_mix])
        nq += 1

    all_kt = list(range(nkt))
    for pair in range(2 if do_attn else 0):
        for qt in range(T // 512):
            attn_block(QT[:, pair, qt * 512:(qt + 1) * 512], 512, all_kt, NCTX + qt * 512, 256 + pair * 128)
        if want_ctx_out:
            attn_block(QTc[:, pair, :], NCTX, [0, 1], 0, 256 + pair * 128)
    P.barrier()
    A.release(m_att)

    tri = A.alloc([2, 128], F32); r_tri = Res()
    bigm = A.alloc([2, 512], F32); r_bigm = Res()
    strict = A.alloc([4, 512], F32); r_strict = Res()
    P.dma("sp", tri, dr["tri"].rearrange("p (a b) -> p a b", a=2), writes=[r_tri])
    P.dma("sp", bigm, dr["bigm"].rearrange("p (a b) -> p a b", a=2), writes=[r_bigm])
    P.dma("sp", strict, dr["strict"].rearrange("p (a b) -> p a b", a=4), writes=[r_strict])
    blk = A.alloc([128], F32); r_blk = Res()
    P.op("dve", "memset", blk, 0.0, writes=[r_blk])
    P.op("dve", "memset", blk[0:64, 0:64], 1.0, writes=[r_blk])
    P.op("dve", "memset", blk[64:128, 64:128], 1.0, writes=[r_blk])
    Oacc = A.alloc([nkt, 256], F32); r_Oacc = [Res() for _ in range(nkt)]
    written = [False] * nkt

    class DS:
        pass
    st = []
    for d_ in range(2):
        z = DS()
        z.R = A.alloc([768], F32); z.r_R = Res()
        z.Kb = A.alloc([4, 64], F32); z.r_Kb = Res()
        z.Qd = A.alloc([4, 64], F32); z.r_Qd = Res()
        z.kd = A.alloc([4, 64], F32); z.r_kd = Res()
        z.G = A.alloc([4], F32); z.r_G = Res()
        z.nG = A.alloc([4], F32); z.r_nG = Res()
        z.eG = A.alloc([4], F32); z.r_eG = Res()
        z.dG = A.alloc([4], F32); z.r_dG = Res()
        z.GL = A.alloc([2], F32); z.r_GL = Res()
        z.TT = A.alloc([8, 128], F32); z.r_TT = Res()
        z.KbP = A.alloc([2, 2, 128], F32); z.r_KbP = Res()
        z.QnP = A.alloc([2, 2, 128], F32); z.r_QnP = Res()
        P.op("pool", "memset", z.KbP, 0.0, writes=[z.r_KbP])
        P.op("pool", "memset", z.QnP, 0.0, writes=[z.r_QnP])
        z.decT = A.alloc([4, 128], F32); z.r_decT = Res()
        z.Bm = A.alloc([4, 128], F32); z.r_Bm = Res()
        z.BmO = A.alloc([4, 128], F32); z.r_BmO = Res()
        z.atT = A.alloc([4, 128], F32); z.r_atT = Res()
        z.Bp = [A.alloc([4, 128], F32) for _ in range(6)]; z.r_Bp = [Res() for _ in range(6)]
        z.BO = A.alloc([4, 128], F32); z.r_BO = Res()
        z.Ac = [A.alloc([4, 128], F32) for _ in range(2)]; z.r_Ac = [Res(), Res()]
        z.r0 = A.alloc([4, 128], F32); z.r_r0 = Res()
        z.r = A.alloc([4, 128], F32); z.r_r = Res()
        z.wc = A.alloc([4, 64], F32); z.r_wc = Res()
        z.wT = A.alloc([2, 128], F32); z.r_wT = Res()
        z.vn = A.alloc([4, 64], F32); z.r_vn = Res()
        z.tS = A.alloc([2, 128], F32); z.r_tS = Res()
        z.S = A.alloc([2, 128], F32); z.r_S = Res()
        P.op("pool", "memset", z.S, 0.0, writes=[z.r_S])
        st.append(z)
    r_b = [Res() for _ in range(8)]

    def dn_visit(c, dlt):
        z = st[dlt]
        last = 127 if dlt == 0 else 0
        t0 = c * 128
        P.dma("sp", z.R, REC[t0:t0 + 128, :], reads=[r_REC], writes=[z.r_R])
        g4 = GB[:, c, dlt * 4:dlt * 4 + 4]
        be4 = GB[:, c, 8 + dlt * 4:8 + dlt * 4 + 4]
        Qn3 = z.R[:, 0:256].rearrange("p (a b) -> p a b", a=4)
        Kn3 = z.R[:, 256:512].rearrange("p (a b) -> p a b", a=4)
        V3 = z.R[:, 512:768].rearrange("p (a b) -> p a b", a=4)
        r3 = z.r
        r03 = z.r0
        bs = 4 * dlt
        pD = PS[:, bs * 512:(bs + 1) * 512]; rD = r_b[bs]
        for h in range(4):
            P.op("pe", "matmul", pD[:, h * 128:(h + 1) * 128], g4[:, h:h + 1].to_broadcast([128, 128]), tri[:, dlt, :],
                 start=True, stop=False, reads=[r_GB, r_tri], writes=[rD], inc=False)
            P.op("pe", "matmul", pD[:, h * 128:(h + 1) * 128], ident, bigm[:, dlt, 0:128], start=False, stop=True,
                 reads=[r_ident, r_bigm], writes=[rD], inc=(h == 3))
        pG = PS[:, (bs + 3) * 512:(bs + 3) * 512 + 4]; rG = r_b[bs + 3]
        P.op("pe", "matmul", pG, tri[:, dlt, :], g4, start=True, stop=True, reads=[r_GB, r_tri], writes=[rG])
        P.op("dve", "tensor_copy", z.G, pG, reads=[rG], writes=[z.r_G])
        P.op("dve", "tensor_scalar", z.nG, z.G, -1.0, None, ALU.mult, reads=[z.r_G], writes=[z.r_nG])
        P.op("act", "activation", z.eG, z.G, AF.Exp, reads=[z.r_G], writes=[z.r_eG])
        pDl = pD.rearrange("p (h i) -> p h i", h=4)[:, :, last]
        P.op("dve", "tensor_tensor", z.dG, pDl, z.G, ALU.subtract, reads=[rD, z.r_G], writes=[z.r_dG])
        P.op("act", "activation", z.dG, z.dG, AF.Exp, reads=[z.r_dG], writes=[z.r_dG])
        pD4 = pD.rearrange("p (a b i) -> p a b i", a=2, b=2)
        P.op("act", "activation", z.GL[0:64, :], pD4[0:64, :, 0, last], AF.Exp, reads=[rD], writes=[z.r_GL])
        P.op("act", "activation", z.GL[64:128, :], pD4[64:128, :, 1, last], AF.Exp, reads=[rD], writes=[z.r_GL])
        for h in range(4):
            P.op("act", "activation", z.decT[:, h, :], pD[:, h * 128:(h + 1) * 128], AF.Exp, bias=z.nG[:, h:h + 1],
                 reads=[rD, z.r_nG], writes=[z.r_decT])
        if dn_stage < 1:
            return
        yield
        P.op("pool", "tensor_tensor", z.Kb, Kn3, be4.unsqueeze(2).to_broadcast([128, 4, 64]), ALU.mult,
             reads=[z.r_R, r_GB], writes=[z.r_Kb])
        P.op("pool", "tensor_tensor", r03[:, :, 0:64], V3, be4.unsqueeze(2).to_broadcast([128, 4, 64]), ALU.mult,
             reads=[z.r_R, r_GB], writes=[z.r_r0])
        P.op("pool", "tensor_tensor", r03[:, :, 64:128], z.Kb, z.eG.unsqueeze(2).to_broadcast([128, 4, 64]), ALU.mult,
             reads=[z.r_Kb, z.r_eG], writes=[z.r_r0])
        P.op("pool", "tensor_tensor", z.Qd, Qn3, z.eG.unsqueeze(2).to_broadcast([128, 4, 64]), ALU.mult,
             reads=[z.r_R, z.r_eG], writes=[z.r_Qd])
        P.op("pool", "tensor_tensor", z.kd, Kn3, z.dG.unsqueeze(2).to_broadcast([128, 4, 64]), ALU.mult,
             reads=[z.r_R, z.r_dG], writes=[z.r_kd])
        if dn_stage < 2:
            return
        yield
        pT = PS[:, (bs + 1) * 512:(bs + 3) * 512]; rT = r_b[bs + 1]; rT2 = r_b[bs + 2]
        Kb2 = z.Kb.rearrange("p a b -> p (a b)"); Qd2 = z.Qd.rearrange("p a b -> p (a b)")
        srcs = [(z.R[:, 256:384], z.r_R), (z.R[:, 384:512], z.r_R), (Kb2[:, 0:128], z.r_Kb), (Kb2[:, 128:256], z.r_Kb),
                (z.R[:, 0:128], z.r_R), (z.R[:, 128:256], z.r_R), (Qd2[:, 0:128], z.r_Qd), (Qd2[:, 128:256], z.r_Qd)]
        for i, (sap, sr) in enumerate(srcs):
            P.op("pe", "transpose", pT[:, i * 128:(i + 1) * 128], sap, ident, reads=[sr, r_ident], writes=[rT, rT2], inc=(i == 7))
        pT3 = pT.rearrange("p (a b) -> p a b", a=8)
        P.op("act", "activation", z.TT[:, 0:2, :], pT3[:, 0:2, :], AF.Copy, reads=[rT, rT2], writes=[z.r_TT])
        P.op("dve", "tensor_copy", z.TT[:, 6:8, :], pT3[:, 6:8, :], reads=[rT, rT2], writes=[z.r_TT])
        for half in range(2):
            lo, hi = half * 64, half * 64 + 64
            P.op("act", "activation", z.KbP[lo:hi, :, half, :], pT3[lo:hi, 2:4, :], AF.Copy, reads=[rT, rT2], writes=[z.r_KbP])
            P.op("dve", "tensor_copy", z.QnP[lo:hi, :, half, :], pT3[lo:hi, 4:6, :], reads=[rT, rT2], writes=[z.r_QnP])
        if dn_stage < 2.3:
            return
        yield
        pKK = PS[:, (bs + 1) * 512:(bs + 2) * 512]; rKK = r_b[bs + 1]
        pKQ = PS[:, (bs + 2) * 512:(bs + 3) * 512]; rKQ = r_b[bs + 2]
        for h in range(4):
            pair, half = h // 2, h % 2
            P.op("pe", "matmul", pKK[:, h * 128:(h + 1) * 128], z.TT[:, 0 + pair, :], z.KbP[:, pair, half, :],
                 start=True, stop=True, reads=[z.r_TT, z.r_KbP], writes=[rKK], inc=(h == 3))
        for h in range(4):
            pair, half = h // 2, h % 2
            P.op("pe", "matmul", pKQ[:, h * 128:(h + 1) * 128], z.TT[:, 0 + pair, :], z.QnP[:, pair, half, :],
                 start=True, stop=True, reads=[z.r_TT, z.r_QnP], writes=[rKQ], inc=(h == 3))
        if dn_stage < 2.6:
            return
        P.op("dve", "tensor_tensor", z.Bm.rearrange("p a b -> p (a b)"), pKK, strict[:, 2 * dlt, :], ALU.mult,
             reads=[rKK, r_strict], writes=[z.r_Bm])
        P.op("dve", "tensor_tensor", z.BmO.rearrange("p a b -> p (a b)"), pKK, strict[:, 2 * dlt + 1, :], ALU.mult,
             reads=[rKK, r_strict], writes=[z.r_BmO])
        P.op("pool", "tensor_tensor", z.Bp[0], z.Bm, z.decT, ALU.mult, reads=[z.r_Bm, z.r_decT], writes=[z.r_Bp[0]])
        P.op("pool", "tensor_tensor", z.BO, z.BmO, z.decT, ALU.mult, reads=[z.r_BmO, z.r_decT], writes=[z.r_BO])
        P.op("dve", "tensor_tensor", z.atT.rearrange("p a b -> p (a b)"), pKQ, z.decT.rearrange("p a b -> p (a b)"), ALU.mult,
             reads=[rKQ, z.r_decT], writes=[z.r_atT])
        if dn_stage < 3:
            return
        yield
        pA = PS[:, (bs + 3) * 512:(bs + 4) * 512]; rA = r_b[bs + 3]
        pB = PS[:, (bs + 2) * 512:(bs + 3) * 512]; rB = r_b[bs + 2]
        for h in range(4):
            P.op("pe", "transpose", pA[:, h * 128:(h + 1) * 128], z.Bp[0][:, h, :], ident,
                 reads=[z.r_Bp[0], r_ident], writes=[rA], inc=(h == 3))
        P.op("act", "activation", z.Ac[0].rearrange("p a b -> p (a b)"), pA, AF.Copy, reads=[rA], writes=[z.r_Ac[0]])
        yield
        for k in range(5):
            cur, nxt = k % 2, (k + 1) % 2
            for h in range(4):
                P.op("pe", "matmul", pB[:, h * 128:(h + 1) * 128], z.Ac[cur][:, h, :], z.Bp[k][:, h, :],
                     start=True, stop=True, reads=[z.r_Ac[cur], z.r_Bp[k]], writes=[rB], inc=(h == 3))
            P.op("act", "activation", z.Bp[k + 1].rearrange("p a b -> p (a b)"), pB, AF.Copy, reads=[rB], writes=[z.r_Bp[k + 1]])
            if k < 4:
                for h in range(4):
                    P.op("pe", "matmul", pA[:, h * 128:(h + 1) * 128], z.Bp[k][:, h, :], z.Ac[cur][:, h, :],
                         start=True, stop=True, reads=[z.r_Ac[cur], z.r_Bp[k]], writes=[rA], inc=(h == 3))
                P.op("act", "activation", z.Ac[nxt].rearrange("p a b -> p (a b)"), pA, AF.Copy, reads=[rA], writes=[z.r_Ac[nxt]])
            yield
        yield
        pR = pKK; rR = rKK
        r2 = r3.rearrange("p a b -> p (a b)")
        r02 = r03.rearrange("p a b -> p (a b)")
        for sweep in range(2):
            for k in range(6):
                src = r03 if (sweep == 0 and k == 0) else r3
                rsrc = z.r_r0 if (sweep == 0 and k == 0) else z.r_r
                for h in range(4):
                    P.op("pe", "matmul", pR[:, h * 128:(h + 1) * 128], z.Bp[k][:, h, :], src[:, h, :], start=True, stop=True,
                         reads=[z.r_Bp[k], rsrc], writes=[rR], inc=(h == 3))
                P.op("dve", "tensor_tensor", r2, src.rearrange("p a b -> p (a b)"), pR,
                     ALU.subtract if k == 0 else ALU.add, reads=[rsrc, rR], writes=[z.r_r])
                yield
            if sweep == 0:
                for h in range(4):
                    P.op("pe", "matmul", pR[:, h * 128:(h + 1) * 128], z.BO[:, h, :], r3[:, h, :], start=True, stop=True,
                         reads=[z.r_BO, z.r_r], writes=[rR], inc=(h == 3))
                P.op("dve", "tensor_tensor", r2, r02, pR, ALU.subtract, reads=[z.r_r0, rR], writes=[z.r_r])
        if dn_stage < 4:
            return
        yield
        P.op("pool", "tensor_copy", z.wc, r3[:, :, 64:128], reads=[z.r_r], writes=[z.r_wc])
        wc2 = z.wc.rearrange("p a b -> p (a b)")
        pW = PS[:, (bs + 3) * 512 + 256:(bs + 4) * 512]; rW = r_b[bs + 3]
        for pr in range(2):
            P.op("pe", "transpose", pW[:, pr * 128:(pr + 1) * 128], wc2[:, pr * 128:(pr + 1) * 128], ident,
                 reads=[z.r_wc, r_ident], writes=[rW], inc=(pr == 1))
        P.op("act", "activation", z.wT.rearrange("p a b -> p (a b)"), pW, AF.Copy, reads=[rW], writes=[z.r_wT])
        if dn_stage < 5:
            return
        yield
        pWS = PS[:, (bs + 2) * 512:(bs + 2) * 512 + 256]; pDS = PS[:, (bs + 2) * 512 + 256:(bs + 3) * 512]; rWS = r_b[bs + 2]
        pO = PS[:, (bs + 3) * 512:(bs + 3) * 512 + 256]
        for pr in range(2):
            P.op("pe", "matmul", pWS[:, pr * 128:(pr + 1) * 128], z.wT[:, pr, :], z.S[:, pr, :], start=True, stop=True,
                 reads=[z.r_wT, z.r_S], writes=[rWS], inc=(pr == 1))
        P.op("dve", "tensor_tensor", z.vn, r3[:, :, 0:64], pWS.rearrange("p (a b) -> p a b", a=4), ALU.subtract,
             reads=[z.r_r, rWS], writes=[z.r_vn])
        vn2 = z.vn.rearrange("p a b -> p (a b)")
        kd2 = z.kd.rearrange("p a b -> p (a b)")
        for pr in range(2):
            P.op("pe", "matmul", pO[:, pr * 128:(pr + 1) * 128], z.TT[:, 6 + pr, :], z.S[:, pr, :], start=True, stop=False,
                 reads=[z.r_TT, z.r_S], writes=[rW], inc=False)
            for hf in range(2):
                h = 2 * pr + hf
                P.op("pe", "matmul", pO[:, h * 64:(h + 1) * 64], z.atT[:, h, :], z.vn[:, h, :], start=False, stop=(hf == 1),
                     reads=[z.r_atT, z.r_vn], writes=[rW], inc=(hf == 1))
        for pr in range(2):
            P.op("pe", "matmul", pDS[:, pr * 128:(pr + 1) * 128], kd2[:, pr * 128:(pr + 1) * 128], vn2[:, pr * 128:(pr + 1) * 128],
                 start=True, stop=True, reads=[z.r_kd, z.r_vn], writes=[rWS], inc=(pr == 1))
        yield
        if not written[c]:
            P.op("act", "activation", Oacc[:, c, :], pO, AF.Copy, reads=[rW], writes=[r_Oacc[c]])
            written[c] = True
        else:
            P.op("dve", "tensor_tensor", Oacc[:, c, :], Oacc[:, c, :], pO, ALU.add, reads=[rW, r_Oacc[c]], writes=[r_Oacc[c]])
        P.op("dve", "tensor_tensor", z.tS, pDS.rearrange("p (a b) -> p a b", a=2), blk.unsqueeze(1).to_broadcast([128, 2, 128]),
             ALU.mult, reads=[rWS, r_blk], writes=[z.r_tS])
        for pr in range(2):
            P.op("dve", "scalar_tensor_tensor", z.S[:, pr, :], z.S[:, pr, :], z.GL[:, pr:pr + 1], z.tS[:, pr, :], ALU.mult, ALU.add,
                 reads=[z.r_S, z.r_GL, z.r_tS], writes=[z.r_S])

    fwd_order = list(range(nkt))
    bwd_order = [1, 0] + list(range(nkt - 1, 1, -1))
    for s_ in range(nkt if do_dn else 0):
        gens = [dn_visit(fwd_order[s_], 0), dn_visit(bwd_order[s_], 1)]
        while gens:
            for g_ in list(gens):
                try:
                    next(g_)
                except StopIteration:
                    gens.remove(g_)

    dnw = A.alloc([256], F32); r_dnw = Res()
    P.dma("sp", dnw, dr["dnw"], writes=[r_dnw])
    zt = [A.alloc([256], F32) for _ in range(2)]; r_zt = [Res(), Res()]
    osq = A.alloc([256], F32); r_osq = Res()
    os4 = A.alloc([4], F32); r_os4 = Res()
    or4 = A.alloc([4], F32); r_or4 = Res()
    om = [A.alloc([256], F32) for _ in range(2)]; r_om = [Res(), Res()]
    c_start = 0 if want_ctx_out else 2
    for c in range(c_start, nkt if do_dn else 0):
        i2 = c % 2
        P.dma("sp", zt[i2], Zd[c * 128:(c + 1) * 128, :], reads=[r_Zd], writes=[r_zt[i2]])
        P.op("act", "activation", zt[i2], zt[i2], AF.Silu, reads=[r_zt[i2]], writes=[r_zt[i2]])
        P.op("dve", "tensor_tensor", osq, Oacc[:, c, :], Oacc[:, c, :], ALU.mult, reads=[r_Oacc[c]], writes=[r_osq])
        P.op("dve", "tensor_reduce", os4, osq.rearrange("p (a b) -> p a b", a=4), AX.X, ALU.add, reads=[r_osq], writes=[r_os4])
        emit_rstd(P, os4, r_os4, or4, r_or4, 1.0 / 64, epst, r_eps)
        P.op("dve", "tensor_tensor", om[i2].rearrange("p (a b) -> p a b", a=4), Oacc[:, c, :].rearrange("p (a b) -> p a b", a=4),
             or4.unsqueeze(2).to_broadcast([128, 4, 64]), ALU.mult, reads=[r_Oacc[c], r_or4], writes=[r_om[i2]])
        P.op("dve", "tensor_tensor", om[i2], om[i2], dnw, ALU.mult, reads=[r_om[i2], r_dnw], writes=[r_om[i2]])
        P.op("dve", "tensor_tensor", om[i2], om[i2], zt[i2], ALU.mult, reads=[r_om[i2], r_zt[i2]], writes=[r_om[i2]])
        P.dma("sp", mix[c * 128:(c + 1) * 128, 0:256], om[i2], reads=[r_om[i2]], writes=[r_mix])

    P.barrier()
    print("stage M emitted: instructions", P.ninst, "arena peak", A.peak)


def emit_F(P, A, PS, dr, blocks, moe, final_norm, F, NE, xrow, mixG, NT, ZP):
    P.barrier()
    A.release(0)

    def bank(i, n=512):
        return PS[:, i * 512:i * 512 + n]

    def bankb(i, n=1024):
        return PS[:, i * 512:(i + 1) * 512].bitcast(BF16)[:, 0:n]

    ident = A.alloc([128], F32); r_ident = Res()
    identb = A.alloc([128], BF16); r_identb = Res()
    P.dma("sp", ident, dr["ident"], writes=[r_ident])
    P.dma("pool", identb, dr["ident"], writes=[r_identb])
    epst = A.alloc([1], F32); r_eps = Res()
    P.op("dve", "memset", epst, EPS, writes=[r_eps])
    sel0, sel1, r_sel = emit_sel(P, A)
    g1b = A.alloc([D], F32); r_g1b = Res()
    weff = A.alloc([D], F32); r_weff = Res()
    shb = A.alloc([D], F32); r_shb = Res()
    g2b = A.alloc([D], F32); r_g2b = Res()
    r_psb = Res()

    def make_mod_tiles(sel):
        mm = A.mark()
        modrow, r_mod = emit_mod(P, A, PS, dr, 4, None)
        nwrow = A.alloc([D], F32, parts=2); r_nw = Res()
        P.dma("sp", nwrow, dr["nw"], writes=[r_nw])
        emit_bcast(P, PS, r_psb, sel, r_sel, modrow[:, 0:1024], r_mod, g1b, r_g1b, bank=7)
        emit_bcast(P, PS, r_psb, sel, r_sel, modrow[:, 2048:3072], r_mod, weff, r_weff, bank=7)
        P.op("dve", "tensor_scalar", weff, weff, 1.0, None, ALU.add, reads=[r_weff], writes=[r_weff])
        emit_bcast(P, PS, r_psb, sel, r_sel, nwrow, r_nw, shb, r_shb, bank=7)
        P.op("dve", "tensor_tensor", weff, weff, shb, ALU.mult, reads=[r_weff, r_shb], writes=[r_weff])
        emit_bcast(P, PS, r_psb, sel, r_sel, modrow[:, 1024:2048], r_mod, shb, r_shb, bank=7)
        emit_bcast(P, PS, r_psb, sel, r_sel, modrow[:, 3072:4096], r_mod, g2b, r_g2b, bank=7)
        P.barrier()
        A.release(mm)

    NBmax = max(b[1] for b in blocks) // 128
    X1 = A.alloc([NBmax, D], F32); r_X1 = [Res() for _ in range(NBmax)]
    h2T = A.alloc([8, NBmax * 128], BF16); r_h2T = Res()
    RW = A.alloc([NBmax, 8], F32); r_RW = Res()
    ss1 = A.alloc([1], F32); r_ss1 = Res()
    rs1 = A.alloc([1], F32); r_rs1 = Res()
    rbt = A.alloc([8], F32); r_rbt = Res()
    P.dma("sp", rbt, dr["rb"], writes=[r_rbt])
    m_ph = A.mark()
    r_out = Res()
    r_bk = [Res() for _ in range(8)]
    cur_sel = None
    for (row0, ntok, is_ctx) in blocks:
        NB = ntok // 128
        sel = sel1 if is_ctx else sel0
        A.release(m_ph)
        if cur_sel is not sel:
            make_mod_tiles(sel)
            cur_sel = sel
        Wout = A.alloc([8, D], BF16); r_Wout = Res()
        P.dma("pool", Wout, dr["wout"].rearrange("(c k) n -> k c n", k=128), writes=[r_Wout])
        Wr = A.alloc([8, 8], F32); r_Wr = Res()
        P.dma("sp", Wr, dr["wr"].rearrange("(c k) n -> k c n", k=128), writes=[r_Wr])
        mb = [A.alloc([D], BF16) for _ in range(2)]; r_mb = [Res(), Res()]
        xt = [A.alloc([D], F32) for _ in range(2)]; r_xt = [Res(), Res()]
        mixT = A.alloc([8, 128], BF16); r_mixT = Res()
        tmpf = A.alloc([D], F32); r_tmpf = Res()
        h2f = A.alloc([D], F32); r_h2f = Res()
        h2b = A.alloc([D], BF16); r_h2b = Res()
        h2Tf = A.alloc([8, 128], F32); r_h2Tf = Res()
        junk = A.alloc([D], F32); r_junk = Res()
        lg = A.alloc([8], F32); r_lg = Res()
        l2 = A.alloc([8], F32); r_l2 = Res()
        eq1 = A.alloc([8], F32); r_eq1 = Res()
        eq2 = A.alloc([8], F32); r_eq2 = Res()
        sm = A.alloc([8], F32); r_sm = Res()
        for s in range(NB):
            r0 = row0 + s * 128
            i2 = s % 2
            P.dma("pool", mb[i2][:, 0:512], mixG[r0:r0 + 128, :], writes=[r_mb[i2]])
            P.dma("pool", mb[i2][:, 512:1024], mixG[NT + r0:NT + r0 + 128, :], writes=[r_mb[i2]])
            P.dma("sp", xt[i2], xrow(r0), writes=[r_xt[i2]])
            pb = bankb(6, 1024)
            for kc in range(8):
                P.op("pe", "transpose", pb[:, kc * 128:(kc + 1) * 128], mb[i2][:, kc * 128:(kc + 1) * 128], identb,
                     reads=[r_mb[i2], r_identb], writes=[r_bk[6]], inc=(kc == 7))
            P.op("act", "activation", mixT, pb.rearrange("p (a b) -> p a b", a=8), AF.Copy, reads=[r_bk[6]], writes=[r_mixT])
            for half in range(2):
                py = bank(4 + half)
                for kc in range(8):
                    P.op("pe", "matmul", py, mixT[:, kc, :], Wout[:, kc, half * 512:(half + 1) * 512],
                         start=(kc == 0), stop=(kc == 7), reads=[r_mixT, r_Wout], writes=[r_bk[4 + half]], inc=(kc == 7))
                hs = slice(half * 512, (half + 1) * 512)
                P.op("dve", "tensor_tensor", tmpf[:, hs], py, g1b[:, hs], ALU.mult, reads=[r_bk[4 + half], r_g1b], writes=[r_tmpf])
                P.op("pool", "tensor_tensor", X1[:, s, hs], tmpf[:, hs], xt[i2][:, hs], ALU.add,
                     reads=[r_tmpf, r_xt[i2]], writes=[r_X1[s]])
            P.op("dve", "scalar_tensor_tensor", junk, X1[:, s, :], 1.0, X1[:, s, :], ALU.mult, ALU.mult, accum_out=ss1,
                 reads=[r_X1[s]], writes=[r_ss1, r_junk])
            emit_rstd(P, ss1, r_ss1, rs1, r_rs1, 1.0 / D, epst, r_eps)
            P.op("dve", "scalar_tensor_tensor", tmpf, X1[:, s, :], rs1, weff, ALU.mult, ALU.mult,
                 reads=[r_X1[s], r_rs1, r_weff], writes=[r_tmpf])
            if moe:
                P.op("pool", "tensor_tensor", h2f, tmpf, shb, ALU.add, reads=[r_tmpf, r_shb], writes=[r_h2f])
                for g in range(2):
                    pt = PS[:, (6 + g) * 512:(7 + g) * 512]
                    for kc in range(4):
                        P.op("pe", "transpose", pt[:, kc * 128:(kc + 1) * 128], h2f[:, (4 * g + kc) * 128:(4 * g + kc + 1) * 128], ident,
                             reads=[r_h2f, r_ident], writes=[r_bk[6 + g]], inc=(kc == 3))
                    P.op("act", "activation", h2Tf[:, 4 * g:4 * g + 4, :], pt.rearrange("p (a b) -> p a b", a=4), AF.Copy,
                         reads=[r_bk[6 + g]], writes=[r_h2Tf])
                P.op("pool", "tensor_copy", h2T[:, :, s * 128:(s + 1) * 128], h2Tf, reads=[r_h2Tf], writes=[r_h2T])
                pl = PS[:, 4 * 512:4 * 512 + 8]
                for kc in range(8):
                    P.op("pe", "matmul", pl, h2Tf[:, kc, :], Wr[:, kc, :], start=(kc == 0), stop=(kc == 7),
                         reads=[r_h2Tf, r_Wr], writes=[r_bk[4]], inc=(kc == 7))
                P.op("dve", "tensor_tensor", lg, pl, rbt, ALU.add, reads=[r_bk[4], r_rbt], writes=[r_lg])
                P.op("dve", "tensor_reduce", sm[:, 0:1], lg, AX.X, ALU.max, reads=[r_lg], writes=[r_sm])
                P.op("dve", "tensor_scalar", eq1, lg, sm[:, 0:1], None, ALU.is_equal, reads=[r_lg, r_sm], writes=[r_eq1])
                P.op("dve", "scalar_tensor_tensor", l2, eq1, -1e30, lg, ALU.mult, ALU.add, reads=[r_eq1, r_lg], writes=[r_l2])
                P.op("dve", "tensor_reduce", sm[:, 1:2], l2, AX.X, ALU.max, reads=[r_l2], writes=[r_sm])
                P.op("dve", "tensor_scalar", eq2, l2, sm[:, 1:2], None, ALU.is_equal, reads=[r_l2, r_sm], writes=[r_eq2])
                P.op("dve", "tensor_tensor", sm[:, 2:3], sm[:, 1:2], sm[:, 0:1], ALU.subtract, reads=[r_sm], writes=[r_sm])
                P.op("act", "activation", sm[:, 3:4], sm[:, 2:3], AF.Exp, reads=[r_sm], writes=[r_sm])
                P.op("dve", "tensor_scalar", sm[:, 4:5], sm[:, 3:4], 1.0, None, ALU.add, reads=[r_sm], writes=[r_sm])
                P.op("dve", "reciprocal", sm[:, 5:6], sm[:, 4:5], reads=[r_sm], writes=[r_sm])
                P.op("dve", "tensor_tensor", sm[:, 6:7], sm[:, 3:4], sm[:, 5:6], ALU.mult, reads=[r_sm], writes=[r_sm])
                P.op("dve", "tensor_scalar", RW[:, s, :], eq1, sm[:, 5:6], None, ALU.mult, reads=[r_eq1, r_sm], writes=[r_RW])
                P.op("dve", "scalar_tensor_tensor", RW[:, s, :], eq2, sm[:, 6:7], RW[:, s, :], ALU.mult, ALU.add,
                     reads=[r_eq2, r_sm, r_RW], writes=[r_RW])
            else:
                P.op("pool", "tensor_tensor", h2b, tmpf, shb, ALU.add, reads=[r_tmpf, r_shb], writes=[r_h2b])
                pt = bankb(7, 1024)
                for kc in range(8):
                    P.op("pe", "transpose", pt[:, kc * 128:(kc + 1) * 128], h2b[:, kc * 128:(kc + 1) * 128], identb,
                         reads=[r_h2b, r_identb], writes=[r_bk[7]], inc=(kc == 7))
                P.op("act", "activation", h2T[:, :, s * 128:(s + 1) * 128], pt.rearrange("p (a b) -> p a b", a=8), AF.Copy,
                     reads=[r_bk[7]], writes=[r_h2T])
        for s in range(NB):
            P.op("pool", "tensor_scalar", X1[:, s, :], X1[:, s, :], 0.5, None, ALU.mult, reads=[r_X1[s]], writes=[r_X1[s]])
        P.barrier()
        A.release(m_ph)
        GS = 4
        wg = [A.alloc([8, GS * 128], BF16) for _ in range(2)]; r_wg = [Res(), Res()]
        wu = [A.alloc([8, GS * 128], BF16) for _ in range(2)]; r_wu = [Res(), Res()]
        wd = [A.alloc([GS, D], BF16) for _ in range(2)]; r_wd = [Res(), Res()]
        sg = [A.alloc([512], F32) for _ in range(2)]; r_sg = [Res(), Res()]
        actT = [A.alloc([GS, 512], BF16) for _ in range(2)]; r_actT = [Res(), Res()]
        nchunk = F // 128
        groups = [(c0, min(GS, nchunk - c0)) for c0 in range(0, nchunk, GS)]
        macros = [(m0, min(512, ntok - m0)) for m0 in range(0, ntok, 512)]
        gi = 0
        mcount = 0
        fcount = 0
        ycount = 0
        for e in range(NE):
            for (c0, gsz) in groups:
                b = gi % 2
                f0 = c0 * 128
                P.dma("pool", wg[b][:, :, 0:gsz * 128], dr["wg"][e, :, f0:f0 + gsz * 128].rearrange("(c k) n -> k c n", k=128),
                      writes=[r_wg[b]])
                P.dma("pool", wu[b][:, :, 0:gsz * 128], dr["wu"][e, :, f0:f0 + gsz * 128].rearrange("(c k) n -> k c n", k=128),
                      writes=[r_wu[b]])
                P.dma("pool", wd[b][:, 0:gsz, :], dr["wd"][e, f0:f0 + gsz * 128, :].rearrange("(c k) n -> k c n", k=128),
                      writes=[r_wd[b]])
                P.op("pool", "tensor_tensor", wd[b][:, 0:gsz, :], wd[b][:, 0:gsz, :], g2b.unsqueeze(1).to_broadcast([128, gsz, D]),
                     ALU.mult, reads=[r_wd[b], r_g2b], writes=[r_wd[b]])
                for (m0, mn) in macros:
                    ab = mcount % 2
                    for fc in range(gsz):
                        pg = PS[:, (0 + fcount % 2) * 512:(0 + fcount % 2) * 512 + mn]; rpg = r_bk[0 + fcount % 2]
                        pu = PS[:, (2 + fcount % 2) * 512:(2 + fcount % 2) * 512 + mn]; rpu = r_bk[2 + fcount % 2]
                        for kc in range(8):
                            P.op("pe", "matmul", pg, wg[b][:, kc, fc * 128:(fc + 1) * 128], h2T[:, kc, m0:m0 + mn],
                                 start=(kc == 0), stop=(kc == 7), reads=[r_wg[b], r_h2T], writes=[rpg], inc=(kc == 7))
                        for kc in range(8):
                            P.op("pe", "matmul", pu, wu[b][:, kc, fc * 128:(fc + 1) * 128], h2T[:, kc, m0:m0 + mn],
                                 start=(kc == 0), stop=(kc == 7), reads=[r_wu[b], r_h2T], writes=[rpu], inc=(kc == 7))
                        sb_ = fcount % 2
                        P.op("act", "activation", sg[sb_][:, 0:mn], pg, AF.Silu, reads=[rpg], writes=[r_sg[sb_]])
                        P.op("dve", "tensor_tensor", actT[ab][:, fc, 0:mn], sg[sb_][:, 0:mn], pu, ALU.mult,
                             reads=[r_sg[sb_], rpu], writes=[r_actT[ab]])
                        fcount += 1
                    for sub in range(mn // 128):
                        sgl = (m0 // 128) + sub
                        for half in range(2):
                            py = PS[:, (4 + ycount % 2) * 512:(5 + ycount % 2) * 512]; rpy = r_bk[4 + ycount % 2]
                            for fc in range(gsz):
                                P.op("pe", "matmul", py, actT[ab][:, fc, sub * 128:(sub + 1) * 128], wd[b][:, fc, half * 512:(half + 1) * 512],
                                     start=(fc == 0), stop=(fc == gsz - 1), reads=[r_actT[ab], r_wd[b]], writes=[rpy], inc=(fc == gsz - 1))
                            hs = slice(half * 512, (half + 1) * 512)
                            scal = RW[:, sgl, e:e + 1] if moe else 1.0
                            P.op("dve", "scalar_tensor_tensor", X1[:, sgl, hs], py, scal, X1[:, sgl, hs], ALU.mult, ALU.add,
                                 reads=[rpy, r_RW, r_X1[sgl]], writes=[r_X1[sgl]])
                            ycount += 1
                    mcount += 1
                gi += 1
        for s in range(NB):
            P.dma("sp", ZP[row0 + s * 128:row0 + (s + 1) * 128, :], X1[:, s, :], reads=[r_X1[s]], writes=[r_out])
        P.barrier()
    print("stage F emitted: instructions", P.ninst, "arena peak", A.peak)


def emit_final(P, A, PS, dr, XN, out, T):
    P.barrier()
    A.release(0)
    epst = A.alloc([1], F32); r_eps = Res()
    P.op("dve", "memset", epst, EPS, writes=[r_eps])
    sel0, sel1, r_sel = emit_sel(P, A)
    fnrow = A.alloc([D], F32, parts=2); r_fn = Res()
    P.dma("sp", fnrow, dr["fnw"], writes=[r_fn])
    fnb = A.alloc([D], F32); r_fnb = Res()
    r_psb = Res()
    emit_bcast(P, PS, r_psb, sel0, r_sel, fnrow, r_fn, fnb, r_fnb, bank=7)
    xt = [A.alloc([D], F32) for _ in range(3)]; r_xt = [Res() for _ in range(3)]
    ot = [A.alloc([D], F32) for _ in range(3)]; r_ot = [Res() for _ in range(3)]
    junk = A.alloc([D], F32); r_junk = Res()
    ss = [A.alloc([1], F32) for _ in range(3)]; r_ss = [Res() for _ in range(3)]
    rs = [A.alloc([1], F32) for _ in range(3)]; r_rs = [Res() for _ in range(3)]
    r_out = Res()
    for s in range(T // 128):
        i = s % 3
        P.dma("sp", xt[i], XN[NCTX + s * 128:NCTX + (s + 1) * 128, :], writes=[r_xt[i]])
        P.op("dve", "scalar_tensor_tensor", junk, xt[i], 1.0, xt[i], ALU.mult, ALU.mult, accum_out=ss[i],
             reads=[r_xt[i]], writes=[r_ss[i], r_junk])
        emit_rstd(P, ss[i], r_ss[i], rs[i], r_rs[i], 1.0 / D, epst, r_eps)
        P.op("dve", "scalar_tensor_tensor", ot[i], xt[i], rs[i], fnb, ALU.mult, ALU.mult,
             reads=[r_xt[i], r_rs[i], r_fnb], writes=[r_ot[i]])
        P.dma("sp", out[s * 128:(s + 1) * 128, :], ot[i], reads=[r_ot[i]], writes=[r_out])
    P.wait_all("sp", [r_out])
    P.barrier()


def emit_cc(P, kind, src, dst, rows_per, reads, writes):
    nc = P.nc
    if "cc" not in P.sems:
        P.sems["cc"] = P.es.enter_context(nc.semaphore("ccsem"))
        P.cccnt = 0
    sem = P.sems["cc"]
    deps = P._deps("pool", reads, writes)
    P._emit_waits("pool", deps)
    nrows = src.shape[0]
    groups = [[0, 1], [2, 3], [4, 5], [6, 7]]
    op = ALU.bypass if kind == "AllGather" else ALU.add
    for i, r0 in enumerate(range(0, nrows, rows_per)):
        n = min(rows_per, nrows - r0)
        if kind == "AllGather":
            o = dst[i]
            o = o[0:2 * n, :]
        else:
            o = dst[r0:r0 + n, :]
        P.cccnt += 1
        P.ninst += 1
        P.q["pool"].append(lambda h, a=src[r0:r0 + n, :], o=o: h.collective_compute(
            kind, op, replica_groups=groups, ins=[a], outs=[o]).then_inc(sem, 1))
    dep = ("cc", P.cccnt)
    for r in reads:
        r.rd["cc"] = dep
    for w in writes:
        w.lw = dep
        w.rd = {}


def build_fused(T, stop_after=99):
    nc = bass.Bass("TRN2", target_bir_lowering=False)
    NT = T + NCTX
    dr = {}

    def din(name, shape):
        dr[name] = nc.dram_tensor(name, list(shape), F32, kind="ExternalInput").ap()

    din("xs", [T, D]); din("cx", [NCTX, D]); din("ccT", [128, 8, 2])
    din("cos", [T, 32]); din("sin", [T, 32])
    din("ident", [128, 128]); din("tri", [128, 256]); din("bigm", [128, 1024]); din("strict", [128, 2048])
    din("fnw", [2, D])
    FH = [1408, 3584]; NEH = [1, 4]
    for l in range(2):
        sfx = "_%d" % l
        din("wmodM" + sfx, [D, 2048]); din("bmodM" + sfx, [2, 2048]); din("nw1" + sfx, [2, D])
        din("wtm" + sfx, [D, 656]); din("wfm" + sfx, [D, 768]); din("convw" + sfx, [128, 6, 5])
        din("gp" + sfx, [128, 16]); din("qkw" + sfx, [128, 320]); din("dnw" + sfx, [128, 256])
        din("wmodF" + sfx, [D, 4096]); din("bmodF" + sfx, [2, 4096]); din("nw2" + sfx, [2, D])
        din("wout" + sfx, [D, D]); din("wg" + sfx, [NEH[l], D, FH[l]]); din("wu" + sfx, [NEH[l], D, FH[l]])
        din("wd" + sfx, [NEH[l], FH[l], D]); din("wr" + sfx, [D, 8]); din("rb" + sfx, [128, 8])
    out = nc.dram_tensor("out", [T, D], F32, kind="ExternalOutput").ap()
    mixA = nc.dram_tensor("mixA", [NT, 512], F32).ap()
    RPG = 1024
    ngc = (NT + RPG - 1) // RPG
    mixGc = nc.dram_tensor("mixGc", [ngc, 2 * RPG, 512], F32).ap()
    Zd = nc.dram_tensor("Zd", [NT, 256], F32).ap()
    REC = nc.dram_tensor("REC", [NT, 768], F32).ap()
    ZP = nc.dram_tensor("ZP", [NT, D], F32).ap()
    XN = [nc.dram_tensor("XN%d" % l, [NT, D], F32).ap() for l in range(2)]

    P = Prog(nc)
    A = Arena(P, 206 * 1024)
    PS = P.ps([128, 4096], F32)
    r_mixA = Res(); r_mixG = Res(); r_ZP = Res(); r_XN = [Res(), Res()]

    class MixG:
        def __getitem__(self, key):
            rs, cs = key
            r0, r1 = rs.start, rs.stop
            rank = r0 // NT
            r0 -= rank * NT; r1 -= rank * NT
            ci = r0 // RPG
            assert (r1 - 1) // RPG == ci
            nrows_c = min(RPG, NT - ci * RPG)
            base = rank * nrows_c + (r0 - ci * RPG)
            return mixGc[ci, base:base + (r1 - r0), cs]
    mixG = MixG()

    for l in range(2):
        sfx = "_%d" % l
        last = l == 1
        drM = dict(ccT=dr["ccT"], cos=dr["cos"], sin=dr["sin"], ident=dr["ident"], tri=dr["tri"], bigm=dr["bigm"],
                   strict=dr["strict"], wmod=dr["wmodM" + sfx], bmod=dr["bmodM" + sfx], nw=dr["nw1" + sfx],
                   wtm=dr["wtm" + sfx], wfm=dr["wfm" + sfx], convw=dr["convw" + sfx], gp=dr["gp" + sfx],
                   qkw=dr["qkw" + sfx], dnw=dr["dnw" + sfx])
        drF = dict(ccT=dr["ccT"], ident=dr["ident"], wmod=dr["wmodF" + sfx], bmod=dr["bmodF" + sfx], nw=dr["nw2" + sfx],
                   wout=dr["wout" + sfx], wg=dr["wg" + sfx], wu=dr["wu" + sfx], wd=dr["wd" + sfx], wr=dr["wr" + sfx],
                   rb=dr["rb" + sfx])
        if l == 0:
            def xrow(t0):
                return dr["cx"][t0:t0 + 128, :] if t0 < NCTX else dr["xs"][t0 - NCTX:t0 - NCTX + 128, :]
        else:
            def xrow(t0):
                return XN[0][t0:t0 + 128, :]
        emit_M(P, A, PS, drM, T, not last, xrow, mixA, Zd, REC)
        P.barrier()
        emit_cc(P, "AllGather", mixA, mixGc, RPG, [r_mixA], [r_mixG])
        P.barrier_cc()
        if stop_after == 2 * l + 1:
            r_o = Res()
            for t0 in range(0, T, 128):
                P.dma("sp", out[t0:t0 + 128, 0:512], mixG[NCTX + t0:NCTX + t0 + 128, :], writes=[r_o])
                P.dma("sp", out[t0:t0 + 128, 512:1024], mixG[NT + NCTX + t0:NT + NCTX + t0 + 128, :], writes=[r_o])
            P.wait_all("sp", [r_o])
            P.barrier()
            break
        blocks = [(NCTX + i * 2048, 2048, False) for i in range(T // 2048)]
        if not last:
            blocks = blocks + [(0, NCTX, True)]
        emit_F(P, A, PS, drF, blocks, 4 if last else 0, False, FH[l], NEH[l], xrow, mixG, NT, ZP)
        P.barrier()
        if last:
            emit_cc(P, "AllReduce", ZP[NCTX:NT, :], XN[l][NCTX:NT, :], 512, [r_ZP], [r_XN[l]])
        else:
            emit_cc(P, "AllReduce", ZP, XN[l], 512, [r_ZP], [r_XN[l]])
        P.barrier_cc()
        if stop_after == 2 * l + 2:
            r_o = Res()
            for t0 in range(0, T, 512):
                P.dma("sp", out[t0:t0 + 512, :], XN[l][NCTX + t0:NCTX + t0 + 512, :], writes=[r_o])
            P.wait_all("sp", [r_o])
            P.barrier()
            break
    if stop_after >= 99:
        emit_final(P, A, PS, dr, XN[1], out, T)
    print("fused: instructions", P.ninst, "arena peak", A.peak)
    P.finish()
    return nc


DN_DIM = 512
OFF_DN_Z = 1536
OFF_DN_A = 2048
OFF_DN_B = 2064
OFF_AT_Q = 2080
OFF_AT_K = 2592
OFF_AT_V = 2720


def consts():
    k = np.arange(128)
    U = (k[:, None] <= k[None, :]).astype(np.float32)
    L = (k[:, None] >= k[None, :]).astype(np.float32)
    tri = np.concatenate([U, L], axis=1)
    j = k[:, None]; i = k[None, :]
    big_f = np.where(i >= j, 0.0, -BIG).astype(np.float32)
    big_b = np.where(i <= j, 0.0, -BIG).astype(np.float32)
    st_f = (i > j).astype(np.float32)
    st_b = (i < j).astype(np.float32)
    bigm = np.concatenate([np.tile(big_f, (1, 4)), np.tile(big_b, (1, 4))], axis=1)
    same = ((i // 64) == (j // 64)).astype(np.float32)
    strict = np.concatenate([np.tile(st_f * same, (1, 4)), np.tile(st_f * (1 - same), (1, 4)),
                             np.tile(st_b * same, (1, 4)), np.tile(st_b * (1 - same), (1, 4))], axis=1)
    return dict(ident=np.eye(128, dtype=np.float32), tri=tri, bigm=bigm, strict=strict)


def rope_tables(T):
    rows = T // 64
    row_pos = np.repeat(np.arange(rows, dtype=np.float32), 64)[:T]
    col_pos = np.tile(np.arange(64, dtype=np.float32), rows)
    n_freq = 16
    freqs = (np.float32(10000.0) ** (-np.arange(n_freq, dtype=np.float32) / np.float32(n_freq))).astype(np.float32)
    ang = np.concatenate([row_pos[:, None] * freqs, col_pos[:, None] * freqs], axis=-1).astype(np.float32)
    return np.cos(ang).astype(np.float32), np.sin(ang).astype(np.float32)


def ccT_of(cb, c_ctx):
    cc = np.stack([cb, c_ctx], axis=0)
    return np.ascontiguousarray(cc.reshape(2, 8, 128).transpose(2, 1, 0)).astype(np.float32)


def prep_M(inp, layer, b, j, x_b, ctx_b, T, cst, cos, sin):
    w_in = inp["w_in"][layer]
    hq = slice(256 * j, 256 * j + 256)
    a_cols = [OFF_DN_A + d * 8 + 4 * j + h for d in range(2) for h in range(4)]
    b_cols = [OFF_DN_B + d * 8 + 4 * j + h for d in range(2) for h in range(4)]
    wtm = np.concatenate([
        w_in[:, OFF_AT_Q + 256 * j: OFF_AT_Q + 256 * j + 256],
        w_in[:, OFF_AT_K + 64 * j: OFF_AT_K + 64 * j + 64],
        w_in[:, OFF_AT_V + 64 * j: OFF_AT_V + 64 * j + 64],
        w_in[:, OFF_DN_Z + 256 * j: OFF_DN_Z + 256 * j + 256],
        w_in[:, a_cols], w_in[:, b_cols]], axis=1)
    wfm = np.concatenate([w_in[:, 0 + 256 * j: 256 * j + 256], w_in[:, 512 + 256 * j: 512 + 256 * j + 256],
                          w_in[:, 1024 + 256 * j: 1024 + 256 * j + 256]], axis=1)
    cw = inp["dn_conv_w"][layer]
    cw_my = np.concatenate([cw[:, 256 * j:256 * j + 256], cw[:, 512 + 256 * j:512 + 256 * j + 256],
                            cw[:, 1024 + 256 * j:1024 + 256 * j + 256]], axis=1)
    convw = np.ascontiguousarray(cw_my.reshape(5, 6, 128).transpose(2, 1, 0))
    dtb = inp["dn_dt_bias"][layer][:, 4 * j:4 * j + 4].reshape(8)
    alog = inp["dn_a_log"][layer][:, 4 * j:4 * j + 4].reshape(8)
    gp = np.tile(np.concatenate([dtb, alog])[None, :], (128, 1))
    qkw = np.tile(np.concatenate([np.tile(inp["at_qnorm_w"][layer], 4), inp["at_knorm_w"][layer]])[None, :], (128, 1))
    dnw = np.tile(np.tile(inp["dn_norm_w"][layer], 4)[None, :], (128, 1))
    m = dict(
        ccT=ccT_of(inp["c"][b], inp["c_ctx"]),
        wmod=inp["w_mod"][layer][:, 0:2048], bmod=np.tile(inp["b_mod"][layer][None, 0:2048], (2, 1)),
        nw=np.tile(inp["norm1_w"][layer][None, :], (2, 1)),
        wtm=wtm, wfm=wfm, convw=convw, gp=gp, qkw=qkw, dnw=dnw, cos=cos, sin=sin)
    m.update(cst)
    if x_b is not None:
        m["xs"] = x_b; m["cx"] = ctx_b
    return {k: np.ascontiguousarray(v, dtype=np.float32) for k, v in m.items()}


def prep_F(inp, layer, b, x_rows, mix_rows, cst, moe, final_norm):
    if moe:
        jm = layer // 2
        wg = inp["moe_w_gate"][jm]; wu = inp["moe_w_up"][jm]; wd = inp["moe_w_down"][jm]
        wr = inp["moe_router_w"][jm]; rb = np.tile(inp["moe_router_b"][jm][None, :], (128, 1))
    else:
        i = layer // 2
        wg = inp["ffn_w_gate"][i][None]; wu = inp["ffn_w_up"][i][None]; wd = inp["ffn_w_down"][i][None]
        wr = np.zeros((1024, 8), np.float32); rb = np.zeros((128, 8), np.float32)
    m = dict(x=x_rows, mixin=mix_rows, ccT=ccT_of(inp["c"][b], inp["c_ctx"]),
             wmod=inp["w_mod"][layer][:, 2048:6144], bmod=np.tile(inp["b_mod"][layer][None, 2048:6144], (2, 1)),
             nw=np.tile(inp["norm2_w"][layer][None, :], (2, 1)), fnw=np.tile(inp["final_norm_w"][None, :], (2, 1)),
             wout=inp["w_out"][layer], wg=wg, wu=wu, wd=wd, wr=wr, rb=rb, ident=cst["ident"])
    return {k: np.ascontiguousarray(v, dtype=np.float32) for k, v in m.items()}


def prep_fused(inp, b, j, T, cst, cos, sin):
    m = dict(xs=inp["x"][b], cx=inp["ctx"][b], ccT=ccT_of(inp["c"][b], inp["c_ctx"]), cos=cos, sin=sin,
             fnw=np.tile(inp["final_norm_w"][None, :], (2, 1)))
    m.update(cst)
    for l in range(2):
        sfx = "_%d" % l
        pm = prep_M(inp, l, b, j, None, None, T, {}, cos, sin)
        m["wmodM" + sfx] = pm["wmod"]; m["bmodM" + sfx] = pm["bmod"]; m["nw1" + sfx] = pm["nw"]
        for k in ("wtm", "wfm", "convw", "gp", "qkw", "dnw"):
            m[k + sfx] = pm[k]
        m["wmodF" + sfx] = inp["w_mod"][l][:, 2048:6144]
        m["bmodF" + sfx] = np.tile(inp["b_mod"][l][None, 2048:6144], (2, 1))
        m["nw2" + sfx] = np.tile(inp["norm2_w"][l][None, :], (2, 1))
        wo = inp["w_out"][l]
        m["wout" + sfx] = np.concatenate([wo[0:256], wo[512:768], wo[256:512], wo[768:1024]], axis=0)
        if l % 2 == 0:
            i = l // 2
            sl = slice(1408 * j, 1408 * (j + 1))
            m["wg" + sfx] = inp["ffn_w_gate"][i][:, sl][None]
            m["wu" + sfx] = inp["ffn_w_up"][i][:, sl][None]
            m["wd" + sfx] = inp["ffn_w_down"][i][sl, :][None]
            m["wr" + sfx] = np.zeros((1024, 8), np.float32)
            m["rb" + sfx] = np.zeros((128, 8), np.float32)
        else:
            jm = l // 2
            own = list(range(4 * j, 4 * j + 4))
            perm = own + [e for e in range(8) if e not in own]
            m["wg" + sfx] = inp["moe_w_gate"][jm][4 * j:4 * j + 4]
            m["wu" + sfx] = inp["moe_w_up"][jm][4 * j:4 * j + 4]
            m["wd" + sfx] = inp["moe_w_down"][jm][4 * j:4 * j + 4]
            m["wr" + sfx] = inp["moe_router_w"][jm][:, perm]
            m["rb" + sfx] = np.tile(inp["moe_router_b"][jm][perm][None, :], (128, 1))
    return {k: np.ascontiguousarray(v, dtype=np.float32) for k, v in m.items()}


def kernel(**inputs):
    from concourse.bass_utils import run_bass_kernel_spmd
    inp = {k: np.asarray(v, dtype=np.float32) for k, v in inputs.items()}
    Bn, T = inp["x"].shape[0], inp["x"].shape[1]
    cst = consts()
    cos, sin = rope_tables(T)
    nc = build_fused(T)
    maps = [prep_fused(inp, core // 2, core % 2, T, cst, cos, sin) for core in range(2 * Bn)]
    res = run_bass_kernel_spmd(nc, maps, core_ids=list(range(2 * Bn))).results
    out = np.stack([res[2 * b]["out"] for b in range(Bn)], axis=0)
    return out.astype(np.float32)
```
